# Optimizing a Trainium2 kernel written in Bass

```python
import math
import functools
import jax
import jax.numpy as jnp
from jax import lax
import numpy as np

D_MODEL = 1024
BATCH = 4
SEQ = 8192
DEPTH = 1

GRID_W = 64
CTX_LEN = 256

D_HYENA = D_MODEL // 2
SHORT_CONV = 3
FILTER_BANDS = 16
FILTER_EMB = 1 + 2 * FILTER_BANDS
FILTER_ORDER = 64
DECAY_TARGET = 1e-2
FAST_DECAY_PCT = 0.3
SLOW_DECAY_PCT = 1.5

HEAD_DIM = 64
D_ATTN = D_MODEL // 2
N_HEADS = D_ATTN // HEAD_DIM
N_KV_HEADS = N_HEADS // 4
GQA_GROUP = N_HEADS // N_KV_HEADS
D_KV = N_KV_HEADS * HEAD_DIM
WINDOW = 128
BLOCK = 128
NB_SIDE = WINDOW // BLOCK
KEYS_PER_BLOCK = (2 * NB_SIDE + 1) * BLOCK
ROPE_BASE = 10000.0
NEG_INF = -1e30

N_BRANCHES = 2
IN_SPLITS = (3 * D_HYENA, 3 * D_HYENA + D_ATTN, 3 * D_HYENA + D_ATTN + D_KV, 3 * D_HYENA + D_ATTN + 2 * D_KV)
IN_COLS = IN_SPLITS[-1] + N_BRANCHES * D_MODEL

N_GROUPS = 4
EXPERTS_PER_GROUP = 4
N_EXPERTS = N_GROUPS * EXPERTS_PER_GROUP
TOP_K = 2
D_EXPERT = D_MODEL // 4

LN_EPS = 1e-5
DEEPNORM_ALPHA = (2.0 * DEPTH) ** 0.25
DEEPNORM_BETA = (8.0 * DEPTH) ** -0.25

kernel_name = 'hybrid_hyena_swa_hmoe_diffusion_block'


def standardize(x):
    xf = x.astype(jnp.float32)
    mu = jnp.mean(xf, axis=-1, keepdims=True)
    var = jnp.mean(jnp.square(xf - mu), axis=-1, keepdims=True)
    return (xf - mu) * lax.rsqrt(var + LN_EPS)


def layer_norm(x, g, b):
    return (standardize(x) * g.astype(jnp.float32) + b.astype(jnp.float32)).astype(x.dtype)


def modulate(x, shift, scale):
    return (standardize(x) * (1.0 + scale.astype(jnp.float32)) + shift.astype(jnp.float32)).astype(x.dtype)


def adaln(cond, w, b):
    m = jax.nn.silu(cond) @ w + b
    return [t[:, None, :] for t in jnp.split(m, 6, axis=-1)]


def axial_rope(rows):
    row = jnp.repeat(jnp.arange(rows, dtype=jnp.float32), GRID_W)
    col = jnp.tile(jnp.arange(GRID_W, dtype=jnp.float32), rows)
    half = HEAD_DIM // 2
    inv_freq = ROPE_BASE ** (-jnp.arange(0, half, 2, dtype=jnp.float32) / half)
    ang = jnp.concatenate([row[:, None] * inv_freq, col[:, None] * inv_freq], axis=-1)
    return jnp.cos(ang), jnp.sin(ang)


def apply_rope(x, cos, sin):
    xf = x.astype(jnp.float32).reshape(x.shape[:-1] + (HEAD_DIM // 2, 2))
    x1, x2 = xf[..., 0], xf[..., 1]
    cs, sn = cos[None, :, None, :], sin[None, :, None, :]
    return jnp.stack([x1 * cs - x2 * sn, x1 * sn + x2 * cs], axis=-1).reshape(x.shape).astype(x.dtype)


def short_conv(u, w, b):
    r = SHORT_CONV // 2
    y = lax.conv_general_dilated(u, w, window_strides=(1,), padding=((r, r),),
                                 dimension_numbers=('NWC', 'WIO', 'NWC'), feature_group_count=u.shape[-1])
    return y + b


def hyena_filters(L, w1, b1, w2, b2, w3, b3, w4, freq):
    f32 = jnp.float32
    t = jnp.linspace(0.0, 1.0, L, dtype=f32)[:, None]
    w = 2.0 * math.pi * jnp.arange(L, dtype=f32)[:, None] / L
    bands = jnp.linspace(1e-4, FILTER_BANDS - 1, FILTER_BANDS, dtype=f32)
    z = jnp.concatenate([t, jnp.cos(bands * w), -jnp.sin(bands * w)], axis=-1)
    fr = freq.astype(f32)
    hdn = jnp.sin(fr * (z @ w1.astype(f32) + b1.astype(f32)))
    hdn = jnp.sin(fr * (hdn @ w2.astype(f32) + b2.astype(f32)))
    hdn = jnp.sin(fr * (hdn @ w3.astype(f32) + b3.astype(f32)))
    h = hdn @ w4.astype(f32)
    max_decay = math.log(DECAY_TARGET) / FAST_DECAY_PCT
    min_decay = math.log(DECAY_TARGET) / SLOW_DECAY_PCT
    deltas = jnp.linspace(min_decay, max_decay, D_HYENA, dtype=f32)
    decay = jnp.exp(-t * jnp.abs(deltas))
    h_fwd = h[:, :D_HYENA] * decay
    h_bwd = h[:, D_HYENA:] * decay
    h2 = jnp.concatenate([h_fwd, h_bwd[::-1]], axis=0)
    return h2 * lax.rsqrt(jnp.sum(h2 * h2, axis=0, keepdims=True) + 1e-6)


def fft_long_conv(v, h2):
    L = v.shape[1]
    vf = jnp.fft.rfft(v.astype(jnp.float32), n=2 * L, axis=1)
    hf = jnp.fft.rfft(h2, n=2 * L, axis=0)
    return jnp.fft.irfft(vf * hf[None], n=2 * L, axis=1)[:, :L]


def hyena_branch(u, conv_w, conv_b, w1, b1, w2, b2, w3, b3, w4, freq, bias):
    u = short_conv(u, conv_w, conv_b)
    x0, x1, v = jnp.split(u, 3, axis=-1)
    h2 = hyena_filters(u.shape[1], w1, b1, w2, b2, w3, b3, w4, freq)
    v = v * x1
    v = fft_long_conv(v, h2).astype(u.dtype) + bias * v
    return v * x0


def windowed_attention(q, k, v, k_ctx, v_ctx, sinks):
    B, L = q.shape[:2]
    nb = L // BLOCK
    pad = NB_SIDE * BLOCK
    qb = q.reshape(B, nb, BLOCK, N_KV_HEADS, GQA_GROUP, HEAD_DIM)

    def neighbourhood(t):
        tb = jnp.pad(t, ((0, 0), (pad, pad), (0, 0), (0, 0))).reshape(B, nb + 2 * NB_SIDE, BLOCK, N_KV_HEADS, HEAD_DIM)
        return jnp.concatenate([tb[:, j:j + nb] for j in range(2 * NB_SIDE + 1)], axis=2)

    k_win, v_win = neighbourhood(k), neighbourhood(v)
    scale = HEAD_DIM ** -0.5
    s_win = jnp.einsum('bnqhgd,bnkhd->bnhgqk', qb, k_win, preferred_element_type=jnp.float32) * scale
    s_ctx = jnp.einsum('bnqhgd,bchd->bnhgqc', qb, k_ctx, preferred_element_type=jnp.float32) * scale
    key_off = jnp.arange(KEYS_PER_BLOCK)[None, :] - pad
    band = jnp.abs(key_off - jnp.arange(BLOCK)[:, None]) <= WINDOW
    k_abs = jnp.arange(nb)[:, None] * BLOCK + key_off
    mask = band[None] & ((k_abs >= 0) & (k_abs < L))[:, None, :]
    s_win = jnp.where(mask[None, :, None, None], s_win, NEG_INF)
    sink = sinks.astype(jnp.float32).reshape(1, 1, N_KV_HEADS, GQA_GROUP, 1, 1)
    m = jnp.maximum(jnp.maximum(s_win.max(-1, keepdims=True), s_ctx.max(-1, keepdims=True)), sink)
    e_win = jnp.exp(s_win - m)
    e_ctx = jnp.exp(s_ctx - m)
    denom = e_win.sum(-1, keepdims=True) + e_ctx.sum(-1, keepdims=True) + jnp.exp(sink - m)
    p_win = (e_win / denom).astype(v.dtype)
    p_ctx = (e_ctx / denom).astype(v.dtype)
    o = jnp.einsum('bnhgqk,bnkhd->bnqhgd', p_win, v_win) + jnp.einsum('bnhgqc,bchd->bnqhgd', p_ctx, v_ctx)
    return o.reshape(B, L, D_ATTN)


def latent_attention(q, k, v, cos, sin, k_ctx, v_ctx, sinks):
    return windowed_attention(apply_rope(q, cos, sin), apply_rope(k, cos, sin), v, k_ctx, v_ctx, sinks)


def context_attention(q, k, v, sinks):
    B, C = q.shape[:2]
    qg = q.reshape(B, C, N_KV_HEADS, GQA_GROUP, HEAD_DIM)
    s = jnp.einsum('bqhgd,bkhd->bhgqk', qg, k, preferred_element_type=jnp.float32) * HEAD_DIM ** -0.5
    sink = jnp.broadcast_to(sinks.astype(jnp.float32).reshape(1, N_KV_HEADS, GQA_GROUP, 1, 1), s.shape[:-1] + (1,))
    p = jax.nn.softmax(jnp.concatenate([s, sink], axis=-1), axis=-1)[..., :C]
    o = jnp.einsum('bhgqk,bkhd->bqhgd', p.astype(v.dtype), v)
    return o.reshape(B, C, D_ATTN)


def context_kv(h_ctx, w_in_l):
    B, C = h_ctx.shape[:2]
    k, v = jnp.split(h_ctx @ w_in_l[:, IN_SPLITS[1]:IN_SPLITS[3]], 2, axis=-1)
    return k.reshape(B, C, N_KV_HEADS, HEAD_DIM), v.reshape(B, C, N_KV_HEADS, HEAD_DIM)


def token_mixer(h, w_in_l, hyena_p, w_bh, w_ba, w_o, attend):
    B, L, _ = h.shape
    u_hy, q, k, v, gates = jnp.split(h @ w_in_l, IN_SPLITS, axis=-1)
    y_hy = hyena_branch(u_hy, *hyena_p)
    y_at = attend(q.reshape(B, L, N_HEADS, HEAD_DIM), k.reshape(B, L, N_KV_HEADS, HEAD_DIM),
                  v.reshape(B, L, N_KV_HEADS, HEAD_DIM))
    g_hy, g_at = jnp.split(jax.nn.sigmoid(gates.astype(jnp.float32)).astype(h.dtype), N_BRANCHES, axis=-1)
    merged = g_hy * (y_hy @ w_bh) + g_at * (y_at @ w_ba)
    return merged @ w_o


def hier_moe(h, w_group, b_group, w_router, b_router, w_gate, w_up, w_down):
    B, L, D = h.shape
    t = h.reshape(B * L, D)
    group_logits = (t @ w_group).astype(jnp.float32) + b_group.astype(jnp.float32)
    group_sel = jnp.argmax(group_logits, axis=-1)
    group_p = jnp.take_along_axis(jax.nn.softmax(group_logits, axis=-1), group_sel[:, None], axis=-1)
    expert_logits = ((t @ w_router).astype(jnp.float32) + b_router.astype(jnp.float32)).reshape(-1, N_GROUPS, EXPERTS_PER_GROUP)
    in_group = jnp.take_along_axis(expert_logits, group_sel[:, None, None], axis=1)[:, 0]
    top_val, top_idx = lax.top_k(in_group, TOP_K)
    top_w = jax.nn.softmax(top_val, axis=-1) * group_p
    expert_id = group_sel[:, None] * EXPERTS_PER_GROUP + top_idx
    gate = jnp.einsum('tk,tke->te', top_w, jax.nn.one_hot(expert_id, N_EXPERTS, dtype=jnp.float32)).astype(t.dtype)
    y = jnp.zeros_like(t)
    for gi in range(N_GROUPS):
        sl = slice(gi * EXPERTS_PER_GROUP, (gi + 1) * EXPERTS_PER_GROUP)
        hid = jax.nn.silu(jnp.einsum('td,edf->tef', t, w_gate[sl])) * jnp.einsum('td,edf->tef', t, w_up[sl])
        y = y + jnp.einsum('tef,efd->td', hid * gate[:, sl, None], w_down[sl])
    return y.reshape(B, L, D)


def setup_inputs(seed: int = 0) -> dict:
    key = jax.random.key(seed)
    ks = iter(jax.random.split(key, 40))

    def nrm(shape, scale=1.0):
        return jax.random.normal(next(ks), shape, jnp.float32) * scale

    D = D_MODEL
    return {
        'x': nrm((BATCH, SEQ, D)),
        'c': nrm((BATCH, D)),
        'ctx': nrm((BATCH, CTX_LEN, D)),
        'c_ctx': nrm((D,)),
        'ada_w': nrm((DEPTH, D, 6 * D), 0.5 * D ** -0.5),
        'ada_b': nrm((DEPTH, 6 * D), 0.02),
        'w_in': nrm((DEPTH, D, IN_COLS), D ** -0.5),
        'hy_conv_w': nrm((DEPTH, SHORT_CONV, 1, 3 * D_HYENA), SHORT_CONV ** -0.5),
        'hy_conv_b': nrm((DEPTH, 3 * D_HYENA), 0.02),
        'hy_w1': nrm((DEPTH, FILTER_EMB, FILTER_ORDER), FILTER_EMB ** -0.5),
        'hy_b1': nrm((DEPTH, FILTER_ORDER), 0.1),
        'hy_w2': nrm((DEPTH, FILTER_ORDER, FILTER_ORDER), FILTER_ORDER ** -0.5),
        'hy_b2': nrm((DEPTH, FILTER_ORDER), 0.1),
        'hy_w3': nrm((DEPTH, FILTER_ORDER, FILTER_ORDER), FILTER_ORDER ** -0.5),
        'hy_b3': nrm((DEPTH, FILTER_ORDER), 0.1),
        'hy_w4': nrm((DEPTH, FILTER_ORDER, 2 * D_HYENA), FILTER_ORDER ** -0.5),
        'hy_freq': 1.0 + nrm((DEPTH, FILTER_ORDER), 0.02),
        'hy_bias': nrm((DEPTH, D_HYENA)),
        'attn_sinks': nrm((DEPTH, N_HEADS)),
        'w_branch_hy': nrm((DEPTH, D_HYENA, D), D_HYENA ** -0.5),
        'w_branch_attn': nrm((DEPTH, D_ATTN, D), D_ATTN ** -0.5),
        'w_out': nrm((DEPTH, D, D), DEEPNORM_BETA * D ** -0.5),
        'ln1_g': 1.0 + nrm((DEPTH, D), 0.02),
        'ln1_b': nrm((DEPTH, D), 0.02),
        'w_group': nrm((DEPTH, D, N_GROUPS), D ** -0.5),
        'b_group': nrm((DEPTH, N_GROUPS), 0.01),
        'w_router': nrm((DEPTH, D, N_EXPERTS), D ** -0.5),
        'b_router': nrm((DEPTH, N_EXPERTS), 0.01),
        'w_gate_e': nrm((DEPTH, N_EXPERTS, D, D_EXPERT), D ** -0.5),
        'w_up_e': nrm((DEPTH, N_EXPERTS, D, D_EXPERT), D ** -0.5),
        'w_down_e': nrm((DEPTH, N_EXPERTS, D_EXPERT, D), DEEPNORM_BETA * D_EXPERT ** -0.5),
        'ln2_g': 1.0 + nrm((DEPTH, D), 0.02),
        'ln2_b': nrm((DEPTH, D), 0.02),
    }


def reference(x, c, ctx, c_ctx, ada_w, ada_b, w_in, hy_conv_w, hy_conv_b, hy_w1, hy_b1, hy_w2, hy_b2,
              hy_w3, hy_b3, hy_w4, hy_freq, hy_bias, attn_sinks, w_branch_hy, w_branch_attn, w_out,
              ln1_g, ln1_b, w_group, b_group, w_router, b_router, w_gate_e, w_up_e, w_down_e, ln2_g, ln2_b):
    L = x.shape[1]
    rows = L // GRID_W
    cos, sin = axial_rope(rows)
    for l in range(DEPTH):
        hyena_p = (hy_conv_w[l], hy_conv_b[l], hy_w1[l], hy_b1[l], hy_w2[l], hy_b2[l],
                   hy_w3[l], hy_b3[l], hy_w4[l], hy_freq[l], hy_bias[l])
        moe_p = (w_group[l], b_group[l], w_router[l], b_router[l], w_gate_e[l], w_up_e[l], w_down_e[l])
        sh1, sc1, g1, sh2, sc2, g2 = adaln(c, ada_w[l], ada_b[l])
        csh1, csc1, cg1, csh2, csc2, cg2 = adaln(c_ctx[None], ada_w[l], ada_b[l])
        h_ctx = modulate(ctx, csh1, csc1)
        k_ctx, v_ctx = context_kv(h_ctx, w_in[l])
        attend_lat = functools.partial(latent_attention, cos=cos, sin=sin, k_ctx=k_ctx, v_ctx=v_ctx, sinks=attn_sinks[l])
        mix = token_mixer(modulate(x, sh1, sc1), w_in[l], hyena_p, w_branch_hy[l], w_branch_attn[l], w_out[l], attend_lat)
        x = layer_norm(DEEPNORM_ALPHA * x + g1 * mix, ln1_g[l], ln1_b[l])
        x = layer_norm(DEEPNORM_ALPHA * x + g2 * hier_moe(modulate(x, sh2, sc2), *moe_p), ln2_g[l], ln2_b[l])
        if l < DEPTH - 1:
            attend_ctx = functools.partial(context_attention, sinks=attn_sinks[l])
            mix_c = token_mixer(h_ctx, w_in[l], hyena_p, w_branch_hy[l], w_branch_attn[l], w_out[l], attend_ctx)
            ctx = layer_norm(DEEPNORM_ALPHA * ctx + cg1 * mix_c, ln1_g[l], ln1_b[l])
            ctx = layer_norm(DEEPNORM_ALPHA * ctx + cg2 * hier_moe(modulate(ctx, csh2, csc2), *moe_p), ln2_g[l], ln2_b[l])
    return x
```

```python
import math
from contextlib import ExitStack

import numpy as np
import ml_dtypes

import concourse.bass as bass
import concourse.mybir as mybir
from concourse.bass_utils import run_bass_kernel_spmd

F32 = mybir.dt.float32
BF16 = mybir.dt.bfloat16
I32 = mybir.dt.int32
U32 = mybir.dt.uint32
ALU = mybir.AluOpType
AF = mybir.ActivationFunctionType
AX = mybir.AxisListType

D = 1024
L = 8192
LO = 4096
NB = 64
NEXT = LO + 256
DH = 512
NKF = 65
GCH = 64
NG = DH // GCH
NE = 16
DE = 256
ALPHA = 2.0 ** 0.25
EPS = 1e-5
TWO_PI = 2.0 * math.pi
C_X1V = 0
C_X0 = 1024
C_Q = 1536
C_QS = 2048
C_K = 2560
C_KS = 2688
C_V = 2816
C_G = 2944
NCOL = 4992


class K:
    def __init__(self, nc, es):
        self.nc = nc
        self.es = es
        self.engs = {'pe': nc.tensor, 'act': nc.scalar, 'dve': nc.vector, 'pool': nc.gpsimd, 'sp': nc.sync}
        self.esem = {}
        self.ecnt = {}
        for e in ['pe', 'act', 'dve', 'pool']:
            self.esem[e] = es.enter_context(nc.semaphore('s_' + e))
            self.ecnt[e] = 0
        self.sems = {}
        for e in self.esem:
            self.sems[id(self.esem[e])] = [self.esem[e], 0]
        self.waited = {}
        self.lastw = {}
        self.readers = {}
        self.dsem = {}
        self.pes = None
        self.nsem = 0

    def begin(self):
        self.pes = ExitStack()
        self.pes.__enter__()
        self.phase = getattr(self, 'phase', 0) + 1

    def end(self):
        self.barrier()
        self.lastw.clear()
        self.readers.clear()
        self.pes.__exit__(None, None, None)
        self.pes = None

    def sbuf(self, name, shape, dt):
        return self.pes.enter_context(self.nc.sbuf_tensor('s%d_%s' % (self.phase, name), shape, dt))

    def psum(self, name, shape, dt):
        return self.pes.enter_context(self.nc.psum_tensor('q%d_%s' % (self.phase, name), shape, dt))

    def _wait(self, e, ev):
        if ev is None:
            return
        sem, val = ev
        if e == 'pe' and sem is self.esem['pe']:
            return
        kk = (e, id(sem))
        if self.waited.get(kk, 0) >= val:
            return
        self.engs[e].wait_ge(sem, val)
        self.waited[kk] = val

    def _deps(self, e, reads, writes):
        for r in reads:
            self._wait(e, self.lastw.get(r))
        for w in writes:
            self._wait(e, self.lastw.get(w))
            for ev in self.readers.get(w, {}).values():
                self._wait(e, ev)

    def _commit(self, ev, reads, writes):
        sid = id(ev[0])
        for r in reads:
            self.readers.setdefault(r, {})[sid] = ev
        for w in writes:
            self.lastw[w] = ev
            self.readers[w] = {}

    def op(self, e, fn, reads=(), writes=()):
        if e == 'pool':
            e = 'dve'
        self._deps(e, reads, writes)
        ins = fn(self.engs[e])
        self.ecnt[e] += 1
        ins.then_inc(self.esem[e], 1)
        ev = (self.esem[e], self.ecnt[e])
        self.sems[id(self.esem[e])][1] = self.ecnt[e]
        self._commit(ev, reads, writes)
        return ev

    def dma(self, q, semkey, out, in_, reads=(), writes=(), slow=False):
        if semkey not in self.dsem:
            s = self.es.enter_context(self.nc.semaphore('d%d' % self.nsem))
            self.nsem += 1
            self.dsem[semkey] = [s, 0]
            self.sems[id(s)] = [s, 0]
        self._deps(q, reads, writes)
        s = self.dsem[semkey]
        s[1] += 16
        if slow:
            ins = self.engs[q].dma_start(out=out, in_=in_, allow_slow_non_contiguous=True)
        else:
            ins = self.engs[q].dma_start(out=out, in_=in_)
        ins.then_inc(s[0], 16)
        ev = (s[0], s[1])
        self.sems[id(s[0])][1] = s[1]
        self._commit(ev, reads, writes)
        return ev

    def barrier(self):
        for e in ['sp', 'pe', 'act', 'dve', 'pool']:
            for sem, val in self.sems.values():
                if val > 0:
                    self._wait(e, (sem, val))


_CONST = {}


def _consts():
    if _CONST:
        return _CONST
    f64 = np.float64
    N = 2 * L
    n1 = np.arange(128, dtype=f64)
    k1 = np.arange(NKF, dtype=f64)
    ang = TWO_PI * np.outer(n1, k1) / 128.0
    _CONST['fc'] = np.concatenate([np.sin(ang), np.cos(ang), -np.sin(ang)], axis=1).astype(np.float32)
    n2 = np.arange(128, dtype=f64)
    k2 = np.arange(128, dtype=f64)
    kk = k1[None, :, None] + 128.0 * k2[None, None, :]
    a2 = TWO_PI * n2[:, None, None] * kk / N
    g = np.stack([np.cos(a2), -np.sin(a2)], axis=2)
    _CONST['gk'] = g.astype(ml_dtypes.bfloat16)
    a3 = TWO_PI * np.outer(k2, n2) / 128.0
    _CONST['cc1'] = np.concatenate([np.cos(a3), np.sin(a3)], axis=1).astype(np.float32)
    _CONST['cc2'] = np.concatenate([-np.sin(a3), np.cos(a3)], axis=1).astype(np.float32)
    t = np.linspace(0.0, 1.0, L, dtype=np.float32)[:, None]
    w = (2.0 * math.pi * np.arange(L, dtype=np.float32)[:, None] / L).astype(np.float32)
    bands = np.linspace(1e-4, 15, 16, dtype=np.float32)
    z = np.concatenate([t, np.cos(bands * w), -np.sin(bands * w)], axis=-1).astype(np.float32)
    _CONST['zz'] = np.ascontiguousarray(np.concatenate([z.T, z[::-1].T], axis=0))
    max_decay = math.log(1e-2) / 0.3
    min_decay = math.log(1e-2) / 1.5
    deltas = np.linspace(min_decay, max_decay, DH, dtype=np.float32)
    tt = np.linspace(0.0, 1.0, L, dtype=np.float32)
    pos = np.arange(L).reshape(64, 128)
    posb = np.concatenate([pos, (L - 1) - pos], axis=0)
    dec = np.exp(-tt[posb][:, :, None] * np.abs(deltas)[None, None, :]).astype(np.float32)
    _CONST['dec'] = np.ascontiguousarray(dec.reshape(128, 128, NG, GCH).transpose(2, 0, 1, 3))
    _CONST['ident'] = np.eye(128, dtype=np.float32)
    sel = np.zeros((16, 16, 128), np.float32)
    for e_ in range(16):
        sel[e_, e_, :] = 1.0
    _CONST['sel'] = sel
    return _CONST


def _core_consts(half):
    f64 = np.float64
    N = 2 * L
    t0 = half * LO
    k1 = np.arange(NKF, dtype=f64)
    wk = np.where((k1 == 0) | (k1 == 64), 1.0, 2.0) / N
    n2 = np.arange(128, dtype=f64)
    n1 = np.arange(32, dtype=f64) + t0 // 128
    n = 128.0 * n1[None, None, :] + n2[None, :, None]
    ang = TWO_PI * k1[:, None, None] * n / N
    mre = (wk[:, None, None] * np.cos(ang)).astype(np.float32)
    mim = (-wk[:, None, None] * np.sin(ang)).astype(np.float32)
    tok = np.arange(t0 - 128, t0 + LO + 128)
    tokc = np.clip(tok, 0, L - 1)
    row = (tokc // 64).astype(np.float32)
    col = (tokc % 64).astype(np.float32)
    half_d = 32
    inv_freq = (10000.0 ** (-np.arange(0, half_d, 2, dtype=np.float32) / half_d)).astype(np.float32)
    ang2 = np.concatenate([row[:, None] * inv_freq, col[:, None] * inv_freq], axis=-1)
    cs, sn = np.cos(ang2), np.sin(ang2)
    d = np.arange(64)
    C = cs[:, d // 2]
    S = sn[:, d // 2] * np.where(d % 2 == 0, -1.0, 1.0)[None, :]
    ropec = np.ascontiguousarray(np.concatenate([C, C], axis=1).T.astype(np.float32))
    ropes = np.ascontiguousarray(np.concatenate([S, S], axis=1).T.astype(np.float32))
    ki = np.arange(128)[:, None]
    qi = np.arange(128)[None, :]
    NEG = -30000.0
    mL = np.where(qi <= ki, 0.0, NEG)
    mR = np.where(ki <= qi, 0.0, NEG)
    mL0 = mL if half == 1 else np.full_like(mL, NEG)
    mR31 = mR if half == 0 else np.full_like(mR, NEG)
    masks = np.stack([np.tile(m, (1, 4)) for m in (mL, mR, mL0, mR31)], axis=1).astype(np.float32)
    hmask = np.tile(np.array([[1.0 if half == 1 else 0.0, 1.0 if half == 0 else 0.0]], np.float32), (128, 1))
    return dict(mre=mre, mim=mim, ropec=ropec, ropes=ropes, masks=masks, hmask=hmask)


def _host_inputs(inp):
    C = _consts()
    g = {k: np.asarray(v) for k, v in inp.items()}
    w_in = g['w_in'][0]
    hy = np.arange(0, 1536)
    x0c, x1c, vc = hy[0:512], hy[512:1024], hy[1024:1536]
    qc = 1536 + np.arange(512)
    kc = 2048 + np.arange(128)
    vac = 2176 + np.arange(128)
    gc = 2304 + np.arange(2048)
    qperm = np.concatenate([np.concatenate([qc[j * 64:(j + 1) * 64], qc[(4 + j) * 64:(5 + j) * 64]]) for j in range(4)])
    sw = np.arange(64) ^ 1
    qsw = np.concatenate([np.concatenate([qc[j * 64:(j + 1) * 64][sw], qc[(4 + j) * 64:(5 + j) * 64][sw]]) for j in range(4)])
    ksw = np.concatenate([kc[0:64][sw], kc[64:128][sw]])
    cols = np.concatenate([x1c, vc, x0c, qperm, qsw, kc, ksw, vac, gc])
    assert cols.shape[0] == NCOL
    w_in_p = np.ascontiguousarray(w_in[:, cols])
    conv_w = g['hy_conv_w'][0][:, 0, :]
    conv_b = g['hy_conv_b'][0]
    hcols = np.concatenate([x1c, vc, x0c])
    cw = np.concatenate([conv_w[:, hcols], conv_b[None, hcols]], axis=0)
    cw = np.ascontiguousarray(cw.reshape(4, 12, 128).transpose(2, 1, 0))
    w1s = np.zeros((66, 128), np.float32)
    w1s[0:33, 0:64] = g['hy_w1'][0]
    w1s[33:66, 64:128] = g['hy_w1'][0]
    w2s = np.zeros((128, 128), np.float32)
    w2s[0:64, 0:64] = g['hy_w2'][0]
    w2s[64:128, 64:128] = g['hy_w2'][0]
    w3s = np.zeros((128, 128), np.float32)
    w3s[0:64, 0:64] = g['hy_w3'][0]
    w3s[64:128, 64:128] = g['hy_w3'][0]
    w4 = g['hy_w4'][0]
    w4s = np.ascontiguousarray(np.concatenate([w4[:, :512], w4[:, 512:]], axis=0))
    hyv = np.stack([np.tile(g[n][0], 2) for n in ('hy_b1', 'hy_b2', 'hy_b3', 'hy_freq')], axis=1).astype(np.float32)
    hbias = np.ascontiguousarray(np.tile(g['hy_bias'][0][None, :], (128, 1)))
    sinks = g['attn_sinks'][0]
    sinks_b = np.ascontiguousarray(np.tile(sinks[None, :], (128, 1)))
    shared = dict(
        ada_w=g['ada_w'][0], ada_b=np.ascontiguousarray(np.tile(g['ada_b'][0][None, :], (2, 1))),
        w_in_p=w_in_p, cw=cw, w1s=w1s, w2s=w2s, w3s=w3s, w4s=w4s, hyv=hyv, hbias=hbias, sinks=sinks_b,
        w_bh=g['w_branch_hy'][0], w_ba=g['w_branch_attn'][0], w_o=g['w_out'][0],
        ln1=np.ascontiguousarray(np.stack([g['ln1_g'][0], g['ln1_b'][0]])),
        ln2=np.ascontiguousarray(np.stack([g['ln2_g'][0], g['ln2_b'][0]])),
        w_gr=np.ascontiguousarray(np.concatenate([g['w_group'][0], g['w_router'][0]], axis=1)),
        b_gr=np.ascontiguousarray(np.tile(np.concatenate([g['b_group'][0], g['b_router'][0]])[None, :], (128, 1))),
        w_ge=g['w_gate_e'][0], w_ue=g['w_up_e'][0], w_de=g['w_down_e'][0],
        fc=C['fc'], gk=C['gk'], cc1=C['cc1'], cc2=C['cc2'], zz=C['zz'], dec=C['dec'], ident=C['ident'], sel=C['sel'],
    )
    maps = []
    x = g['x']
    for core in range(8):
        b, half = core // 2, core % 2
        t0 = half * LO
        xe = np.zeros((NEXT, D), np.float32)
        lo, hi = max(0, t0 - 128), min(L, t0 + LO + 128)
        xe[lo - (t0 - 128):hi - (t0 - 128)] = x[b, lo:hi]
        cc = np.stack([g['c'][b], g['c_ctx']], axis=1).reshape(8, 128, 2).transpose(1, 0, 2)
        m = dict(shared)
        m.update(xfull=np.ascontiguousarray(x[b]), xext=xe, cc=np.ascontiguousarray(cc.astype(np.float32)),
                 ctx=np.ascontiguousarray(g['ctx'][b]))
        m.update(_core_consts(half))
        maps.append(m)
    return maps


IN_SPECS = dict(
    xfull=([L, D], F32), xext=([NEXT, D], F32), cc=([128, 8, 2], F32), ctx=([256, D], F32),
    ada_w=([D, 6 * D], F32), ada_b=([2, 6 * D], F32), w_in_p=([D, NCOL], F32), cw=([128, 12, 4], F32),
    w1s=([66, 128], F32), w2s=([128, 128], F32), w3s=([128, 128], F32), w4s=([128, 512], F32),
    hyv=([128, 4], F32), hbias=([128, 512], F32), sinks=([128, 8], F32),
    w_bh=([512, D], F32), w_ba=([512, D], F32), w_o=([D, D], F32), ln1=([2, D], F32), ln2=([2, D], F32),
    w_gr=([D, 20], F32), b_gr=([128, 20], F32), w_ge=([NE, D, DE], F32), w_ue=([NE, D, DE], F32), w_de=([NE, DE, D], F32),
    fc=([128, 3 * NKF], F32), gk=([128, NKF, 2, 128], BF16), cc1=([128, 256], F32), cc2=([128, 256], F32),
    zz=([66, L], F32), dec=([NG, 128, 128, GCH], F32), ident=([128, 128], F32),
    mre=([NKF, 128, 32], F32), mim=([NKF, 128, 32], F32), ropec=([128, NEXT], F32), ropes=([128, NEXT], F32),
    masks=([128, 4, 512], F32), sel=([16, 16, 128], F32), hmask=([128, 2], F32),
)
SCRATCH = dict(
    modv=([2, 6 * D], F32), vx=([DH, 17 * 512], BF16), yc=([DH, LO], BF16), gt=([2 * D, LO], BF16),
    x1=([LO, D], F32), kvc=([256, 256], F32), wgu=([NE, 2, 128, 8 * DE], BF16), wdb=([NE, 128, 2 * D], BF16),
    dbg_yat=([LO, 512], BF16), dbg_mix=([LO, D], F32), dbg_yh=([512, LO], BF16),
    dbg_qr=([128, 4 * LO], BF16), dbg_kr=([128, NEXT], BF16), dbg_vt=([128, 34 * 130], BF16), dbg_kc=([128, 256], BF16), dbg_vc=([128, 260], BF16),
)


def ln_tile(k, xt, xn, st, mv, rstd, kx, sl, kout=None):
    for h in range(2):
        k.op('dve', lambda e: e.bn_stats(out=st[:, h, :], in_=xt[:, h * 512:(h + 1) * 512]), reads=[kx], writes=[('st', sl)])
    k.op('dve', lambda e: e.bn_aggr(out=mv[:], in_=st[:].rearrange("p a b -> p (a b)")), reads=[('st', sl)], writes=[('mv', sl)])
    k.op('act', lambda e: e.activation(out=rstd[:], in_=mv[:, 1:2], func=AF.Sqrt, bias=EPS, scale=1.0),
         reads=[('mv', sl)], writes=[('rstd', sl)])
    k.op('dve', lambda e: e.reciprocal(out=rstd[:], in_=rstd[:]), reads=[('rstd', sl)], writes=[('rstd', sl)])
    k.op('dve', lambda e: e.tensor_scalar(out=xn[:], in0=xt[:], scalar1=mv[:, 0:1], scalar2=rstd[:, 0:1],
                                          op0=ALU.subtract, op1=ALU.mult),
         reads=[kx, ('mv', sl), ('rstd', sl)], writes=[kout if kout is not None else ('xn', sl)])


def load_pvec(k, dst, src_row, key, q='sp'):
    k.dma(q, key, dst, src_row.rearrange("(kc p) -> p kc", p=128), writes=[key], slow=True)


def phase1(k, T):
    k.begin()
    cs = k.sbuf('cs', [128, 8, 2], F32)
    csl = k.sbuf('csl', [128, 8, 2], F32)
    ab = k.sbuf('ab', [2, 6 * D], F32)
    mo = k.sbuf('mo', [2, 6 * D], F32)
    wb = [k.sbuf('aw%d' % i, [128, 8, 512], F32) for i in range(2)]
    ps = [k.psum('p1_%d' % i, [128, 512], F32) for i in range(2)]
    k.dma('sp', 'cs', cs[:], T['cc'], writes=['cs'])
    k.dma('sp', 'ab', ab[:], T['ada_b'], writes=['ab'])
    k.op('act', lambda e: e.activation(out=csl[:], in_=cs[:], func=AF.Silu), reads=['cs'], writes=['csl'])
    for n in range(12):
        s = n % 2
        k.dma('sp', ('aw', s), wb[s][:],
              T['ada_w'][:, n * 512:(n + 1) * 512].rearrange("(kc p) n -> p kc n", p=128), writes=[('aw', s)])
        for kc in range(8):
            k.op('pe', lambda e: e.matmul(ps[s][0:2, :], lhsT=csl[:, kc, :], rhs=wb[s][:, kc, :], start=(kc == 0), stop=(kc == 7)),
                 reads=['csl', ('aw', s)], writes=[('p1', s)])
        k.op('dve', lambda e: e.tensor_tensor(out=mo[:, n * 512:(n + 1) * 512], in0=ps[s][0:2, :], in1=ab[:, n * 512:(n + 1) * 512], op=ALU.add),
             reads=[('p1', s), 'ab'], writes=['mo'])
    k.dma('sp', 'modv', T['modv'], mo[:], reads=['mo'], writes=['modv_d'])
    k.end()


def phase2(k, T):
    k.begin()
    identf = k.sbuf('identf', [128, 128], F32)
    ident = k.sbuf('ident', [128, 128], BF16)
    s1p = k.sbuf('s1p', [128, 8], F32)
    sh1p = k.sbuf('sh1p', [128, 8], F32)
    wst = [k.sbuf('wst%d' % i, [128, 8, 512], F32) for i in range(2)]
    wA = k.sbuf('wA', [128, 8, 1024], BF16)
    cw = k.sbuf('cw', [128, 12, 4], F32)
    xt = [k.sbuf('xt%d' % i, [128, D], F32) for i in range(2)]
    xn = [k.sbuf('xn%d' % i, [128, D], BF16) for i in range(2)]
    st = [k.sbuf('st%d' % i, [128, 2, 6], F32) for i in range(2)]
    mv = [k.sbuf('mv%d' % i, [128, 2], F32) for i in range(2)]
    rstd = [k.sbuf('rstd%d' % i, [128, 1], F32) for i in range(2)]
    hT = [k.sbuf('hT%d' % i, [128, 8, 512], BF16) for i in range(2)]
    ub = [k.sbuf('ub%d' % i, [128, 8, 514], F32) for i in range(2)]
    tc = k.sbuf('tc', [128, 8, 512], F32)
    vxs = [k.sbuf('vxs%d' % i, [128, 4, 512], BF16) for i in range(2)]
    pT = [k.psum('pT%d' % i, [128, 8, 128], BF16) for i in range(2)]
    pu = [k.psum('pu%d' % i, [128, 512], F32) for i in range(2)]

    cstb = [k.sbuf('cst%d' % i, [128, 2048], F32) for i in range(4)]
    cbf = [k.sbuf('cbf%d' % i, [128, 2048], BF16) for i in range(6)]
    stg_bufs = [(cstb[i][:], ('cst', i)) for i in range(4)] + [(wst[j][:].rearrange("p a b -> p (a b)")[:, 0:2048], ('wst', j)) for j in range(2)]
    jobs = []
    for e_ in range(NE):
        jobs.append((T['w_ge'][e_].rearrange("(kc p) f -> p kc f", p=128), 8, T['wgu'][e_, 0]))
        jobs.append((T['w_ue'][e_].rearrange("(kc p) f -> p kc f", p=128), 8, T['wgu'][e_, 1]))
        jobs.append((T['w_de'][e_].rearrange("(fc p) d -> p fc d", p=128), 2, T['wdb'][e_]))
    jl = [0]
    jc = [0]
    outq = []

    def conv_load():
        if jl[0] >= len(jobs):
            return
        src, a_, dst = jobs[jl[0]]
        buf, key = stg_bufs[jl[0] % 6]
        jl[0] += 1
        k.dma('sp', key, buf.rearrange("p (a b) -> p a b", a=a_), src, writes=[key])

    def conv_cast():
        while len(outq) > 2:
            i_, dst_ = outq.pop(0)
            k.dma('sp', ('cbo', i_), dst_, cbf[i_][:], reads=[('cbf', i_)], writes=['wconv_d'])
        if jc[0] >= jl[0]:
            return
        src, a_, dst = jobs[jc[0]]
        i = jc[0] % 6
        buf, key = stg_bufs[i]
        jc[0] += 1
        k.op('act', lambda en: en.activation(out=cbf[i][:], in_=buf, func=AF.Copy), reads=[key], writes=[('cbf', i)])
        outq.append((i, dst))

    def conv_flush():
        while jc[0] < len(jobs):
            if jl[0] < len(jobs):
                conv_load()
            conv_cast()
        while outq:
            i_, dst_ = outq.pop(0)
            k.dma('sp', ('cbo', i_), dst_, cbf[i_][:], reads=[('cbf', i_)], writes=['wconv_d'])

    k.dma('sp', 'identf', identf[:], T['ident'], writes=['identf'])
    k.op('act', lambda e: e.activation(out=ident[:], in_=identf[:], func=AF.Copy), reads=['identf'], writes=['ident'])
    load_pvec(k, sh1p[:], T['modv'][0, 0:D], 'sh1p')
    load_pvec(k, s1p[:], T['modv'][0, D:2 * D], 's1p')
    k.op('dve', lambda e: e.tensor_scalar(out=s1p[:], in0=s1p[:], scalar1=1.0, scalar2=None, op0=ALU.add), reads=['s1p'], writes=['s1p'])
    k.dma('sp', 'cw', cw[:], T['cw'], writes=['cw'])
    shb = k.sbuf('shb', [128, 8], BF16)
    wAu = k.sbuf('wAu', [128, 8, 1024], BF16)
    c0 = k.sbuf('c0', [128, 8], F32)
    k.op('dve', lambda e: e.tensor_copy(out=shb[:], in_=sh1p[:]), reads=['sh1p'], writes=['shb'])
    for j in range(2):
        k.dma('sp', ('wst', j), wst[j][:], T['w_in_p'][:, C_X1V + j * 512:C_X1V + (j + 1) * 512].rearrange("(kc p) n -> p kc n", p=128),
              writes=[('wst', j)])
        k.op('dve', lambda e: e.tensor_copy(out=wAu[:, :, j * 512:(j + 1) * 512], in_=wst[j][:]), reads=[('wst', j)], writes=['wAu'])
        for kc in range(8):
            k.op('act', lambda e: e.activation(out=wA[:, kc, j * 512:(j + 1) * 512], in_=wst[j][:, kc, :], func=AF.Copy, scale=s1p[:, kc:kc + 1]),
                 reads=[('wst', j), 's1p'], writes=['wA'])
    for cc in range(8):
        for kc in range(8):
            k.op('pe', lambda e: e.matmul(pu[0][:, cc:cc + 1], lhsT=wAu[:, kc, cc * 128:(cc + 1) * 128], rhs=shb[:, kc:kc + 1], start=(kc == 0), stop=(kc == 7)),
                 reads=['wAu', 'shb'], writes=[('pu', 0)])
    k.op('dve', lambda e: e.tensor_copy(out=c0[:], in_=pu[0][:, 0:8]), reads=[('pu', 0)], writes=['c0'])
    k.op('pool', lambda e: e.memset(ub[0][:, :, 0:2], 0.0), writes=[('ub', 0, c_) for c_ in range(8)])

    xv = T['vx'].rearrange("(cc p) t -> p cc t", p=128)
    ti = [0]

    pu = pu + [k.psum('pu%d' % i, [128, 512], F32) for i in range(2, 4)]

    def prep_ln(i, j):
        sl = ti[0] % 2
        ti[0] += 1
        r0 = i * 512 + j * 128
        k.dma('sp', ('xt', sl), xt[sl][:], T['xfull'][r0:r0 + 128, :], writes=[('xt', sl)])
        ln_tile(k, xt[sl], xn[sl], st[sl], mv[sl], rstd[sl], ('xt', sl), sl)
        return sl

    def prep_tr(sl):
        for kc in range(8):
            k.op('pe', lambda e: e.transpose(out=pT[sl][:, kc, :], in_=xn[sl][:, kc * 128:(kc + 1) * 128], identity=ident[:]),
                 reads=[('xn', sl), 'ident'], writes=[('pT', sl)])

    def prep_mod(i, j, sl):
        s = i % 2
        k.op('act', lambda e: e.activation(out=hT[s][:, :, j * 128:(j + 1) * 128], in_=pT[sl][:, :, :], func=AF.Copy),
             reads=[('pT', sl)], writes=[('hT', s)])

    def conv_cc(s, cc):
        k.op('act', lambda e: e.activation(out=tc[:, cc, :], in_=ub[s][:, cc, 1:513], func=AF.Identity,
                                           scale=cw[:, cc, 1:2], bias=cw[:, cc, 3:4]),
             reads=[('ub', s, cc), 'cw'], writes=[('tc', cc)])
        k.op('dve', lambda e: e.scalar_tensor_tensor(out=tc[:, cc, :], in0=ub[s][:, cc, 0:512], scalar=cw[:, cc, 0:1], in1=tc[:, cc, :],
                                                     op0=ALU.mult, op1=ALU.add),
             reads=[('ub', s, cc), 'cw', ('tc', cc)], writes=[('tc', cc)])
        k.op('dve', lambda e: e.scalar_tensor_tensor(out=tc[:, cc, :], in0=ub[s][:, cc, 2:514], scalar=cw[:, cc, 2:3], in1=tc[:, cc, :],
                                                     op0=ALU.mult, op1=ALU.add),
             reads=[('ub', s, cc), 'cw', ('tc', cc)], writes=[('tc', cc)])

    for _ in range(3):
        conv_load()
    for j in range(4):
        sl0 = prep_ln(0, j)
        prep_tr(sl0)
        prep_mod(0, j, sl0)
    for i in range(17):
        s = i % 2
        if i < 16:
            for pr in range(4):
                prep = i + 1 < 16
                if prep:
                    slp = prep_ln(i + 1, pr)
                for cc in (2 * pr, 2 * pr + 1):
                    ps_ = cc % 4
                    for kc in range(8):
                        k.op('pe', lambda e: e.matmul(pu[ps_][:], lhsT=wA[:, kc, cc * 128:(cc + 1) * 128], rhs=hT[s][:, kc, :],
                                                      start=(kc == 0), stop=(kc == 7)),
                             reads=['wA', ('hT', s)], writes=[('pu', ps_)])
                if prep:
                    prep_tr(slp)
                for cc in (2 * pr, 2 * pr + 1):
                    ps_ = cc % 4
                    k.op('act', lambda e: e.activation(out=ub[s][:, cc, 2:514], in_=pu[ps_][:], func=AF.Identity, bias=c0[:, cc:cc + 1], scale=1.0),
                         reads=[('pu', ps_), 'c0'], writes=[('ub', s, cc)])
                    conv_cc(s, cc)
                if prep:
                    prep_mod(i + 1, pr, slp)
                if pr < 3:
                    conv_cast()
                    conv_load()
        else:
            k.op('pool', lambda e: e.memset(ub[s][:, :, 2:514], 0.0), writes=[('ub', s, c_) for c_ in range(8)])
            for cc in range(8):
                conv_cc(s, cc)
        k.op('dve', lambda e: e.tensor_tensor(out=vxs[s][:], in0=tc[:, 0:4, :], in1=tc[:, 4:8, :], op=ALU.mult),
             reads=[('tc', c_) for c_ in range(8)], writes=[('vxs', s)])
        k.dma('sp', ('vxo', s), xv[:, :, i * 512:(i + 1) * 512], vxs[s][:], reads=[('vxs', s)], writes=['vx_d'])
        if i < 16:
            k.op('pool', lambda e: e.tensor_copy(out=ub[1 - s][:, :, 0:2], in_=ub[s][:, :, 512:514]), reads=[('ub', s, c_) for c_ in range(8)], writes=[('ub', 1 - s, c_) for c_ in range(8)])
    conv_flush()
    k.end()


PHASES = {}
DEBUG = False


def build_nc(test=None):
    nc = bass.Bass("TRN2", target_bir_lowering=False)
    T = {}
    ext_in = set(test[1]) if test else set()
    ext_out = set(test[2]) if test else set()
    for n, (shape, dt) in IN_SPECS.items():
        T[n] = nc.dram_tensor(n, shape, dt, kind="ExternalInput").ap()
    for n, (shape, dt) in SCRATCH.items():
        kind = "ExternalInput" if n in ext_in else ("ExternalOutput" if n in ext_out else "Internal")
        T[n] = nc.dram_tensor(n, shape, dt, kind=kind).ap()
    T['out'] = nc.dram_tensor("out", [LO, D], F32, kind="ExternalOutput").ap()
    with ExitStack() as es:
        k = K(nc, es)
        names = test[0] if test else ['phase1', 'phase2', 'phase3', 'phase4', 'phase5']
        for n in names:
            PHASES[n](k, T)
        k.barrier()
    return nc


PHASES.update(phase1=phase1, phase2=phase2)


def phase3(k, T):
    k.begin()
    HD = k.sbuf('HD', [128, 128, 128], BF16)
    gk = k.sbuf('gk', [128, NKF, 2, 128], BF16)
    fc = k.sbuf('fc', [128, 3 * NKF], BF16)
    cc1 = k.sbuf('cc1', [128, 256], BF16)
    cc2 = k.sbuf('cc2', [128, 256], BF16)
    mre = k.sbuf('mre', [128, 128, 32], BF16)
    mim = k.sbuf('mim', [128, 128, 32], BF16)
    w4s = k.sbuf('w4s', [128, 512], BF16)
    hbias = k.sbuf('hbias', [128, 512], F32)
    ones = k.sbuf('ones', [128, 128], F32)
    banks = [k.psum('bk%d' % i, [128, 512], F32) for i in range(8)]

    def bank(i):
        return banks[i], ('bk', i)

    k.dma('sp', 'gk', gk[:], T['gk'], writes=['gk'])
    k.dma('sp', 'hbias', hbias[:], T['hbias'], writes=['hbias'])
    k.op('pool', lambda e: e.memset(ones[:], 1.0), writes=['ones'])
    k.op('pool', lambda e: e.memset(HD[:], 0.0), writes=['HD'])

    outer = k.pes
    k.pes = ExitStack()
    k.pes.__enter__()
    stg = k.sbuf('stg', [128, 4096], F32)
    w1s = k.sbuf('w1s', [66, 128], F32)
    w2s = k.sbuf('w2s', [128, 128], F32)
    w3s = k.sbuf('w3s', [128, 128], F32)
    hyv = k.sbuf('hyv', [128, 4], F32)
    fr2 = k.sbuf('fr2', [128, 1], F32)
    frb = k.sbuf('frb', [128, 3], F32)
    zc = [k.sbuf('zc%d' % i, [66, 512], F32) for i in range(2)]
    rrs = [k.sbuf('rr%d' % i, [128, 512], F32) for i in range(2)]
    r2s = [k.sbuf('r2%d' % i, [128, 512], F32) for i in range(2)]
    hhs = [[k.sbuf('hh%d_%d' % (i, j), [128, 512], F32) for j in range(2)] for i in range(2)]

    def load_cast(dst, src, n, key):
        k.dma('sp', 'stg', stg[:src.shape[0], 0:n], src, writes=['stg'])
        k.op('act', lambda e: e.activation(out=dst, in_=stg[:src.shape[0], 0:n], func=AF.Copy), reads=['stg'], writes=[key])

    load_cast(fc[:], T['fc'], 3 * NKF, 'fc')
    load_cast(cc1[:], T['cc1'], 256, 'cc1')
    load_cast(cc2[:], T['cc2'], 256, 'cc2')
    load_cast(w4s[:], T['w4s'], 512, 'w4s')
    load_cast(mre[0:NKF].rearrange("p a b -> p (a b)"), T['mre'].rearrange("p a b -> p (a b)"), 4096, 'mre')
    load_cast(mim[0:NKF].rearrange("p a b -> p (a b)"), T['mim'].rearrange("p a b -> p (a b)"), 4096, 'mim')
    k.dma('sp', 'w1s', w1s[:], T['w1s'], writes=['w1s'])
    k.dma('sp', 'w2s', w2s[:], T['w2s'], writes=['w2s'])
    k.dma('sp', 'w3s', w3s[:], T['w3s'], writes=['w3s'])
    k.dma('sp', 'hyv', hyv[:], T['hyv'], writes=['hyv'])
    k.op('dve', lambda e: e.tensor_scalar(out=fr2[:], in0=hyv[:, 3:4], scalar1=1.0 / TWO_PI, scalar2=None, op0=ALU.mult),
         reads=['hyv'], writes=['fr2'])
    k.op('dve', lambda e: e.tensor_scalar(out=frb[:], in0=hyv[:, 0:3], scalar1=fr2[:, 0:1], scalar2=None, op0=ALU.mult),
         reads=['hyv', 'fr2'], writes=['frb'])

    def sin_layer(li, pin, kin, out_fn, s):
        rr, r2 = rrs[s], r2s[s]
        k.op('dve', lambda e: e.tensor_scalar(out=rr[:], in0=pin, scalar1=fr2[:, 0:1], scalar2=frb[:, li:li + 1], op0=ALU.mult, op1=ALU.add),
             reads=[kin, 'fr2', 'frb'], writes=[('rr', s)])
        k.op('dve', lambda e: e.scalar_tensor_tensor(out=r2[:], in0=rr[:], scalar=-0.5, in1=rr[:], op0=ALU.is_lt, op1=ALU.add),
             reads=[('rr', s)], writes=[('r2', s)])
        k.op('dve', lambda e: e.scalar_tensor_tensor(out=r2[:], in0=rr[:], scalar=0.5, in1=r2[:], op0=ALU.is_gt, op1=ALU.subtract),
             reads=[('rr', s), ('r2', s)], writes=[('r2', s)])
        out_fn()

    def mlp_layer(ch, li):
        s = ch % 2
        r2 = r2s[s]
        hh = hhs[s]
        bk_, kb_ = bank(2 * li + s)
        if li == 0:
            k.dma('sp', ('zc', s), zc[s][:], T['zz'][:, ch * 512:(ch + 1) * 512], writes=[('zc', s)])
            k.op('pe', lambda e: e.matmul(bk_[:], lhsT=w1s[:], rhs=zc[s][:], start=True, stop=True), reads=['w1s', ('zc', s)], writes=[kb_])
        elif li == 1:
            k.op('pe', lambda e: e.matmul(bk_[:], lhsT=w2s[:], rhs=hh[0][:], start=True, stop=True), reads=['w2s', ('hh', s, 0)], writes=[kb_])
        else:
            k.op('pe', lambda e: e.matmul(bk_[:], lhsT=w3s[:], rhs=hh[1][:], start=True, stop=True), reads=['w3s', ('hh', s, 1)], writes=[kb_])

        def out_fn():
            if li < 2:
                k.op('act', lambda e: e.activation(out=hh[li][:], in_=r2[:], func=AF.Sin, scale=-TWO_PI), reads=[('r2', s)], writes=[('hh', s, li)])
            else:
                for hf in range(2):
                    m0 = 4 * ch + 64 * hf
                    k.op('act', lambda e: e.activation(out=HD[64 * hf:64 * hf + 64, :, m0:m0 + 4].rearrange("p n m -> p m n"),
                                                       in_=r2[64 * hf:64 * hf + 64, :].rearrange("p (m n) -> p m n", m=4),
                                                       func=AF.Sin, scale=-TWO_PI), reads=[('r2', s)], writes=['HD'])
        sin_layer(li, bk_[:], kb_, out_fn, s)

    for ch in range(0, 16, 2):
        for li in range(3):
            mlp_layer(ch, li)
            mlp_layer(ch + 1, li)
    k.barrier()
    k.pes.__exit__(None, None, None)
    k.pes = outer

    dec = [k.sbuf('dec%d' % i, [128, 32, GCH], F32) for i in range(2)]
    XH = k.sbuf('XH', [128, GCH, 128], BF16)
    U = k.sbuf('U', [128, 16384], BF16)
    A_sb = U[:, 0:3 * NKF * GCH].rearrange("p (c a k) -> p c a k", c=GCH, a=3)
    D_sb = U[:, 0:2 * 128 * GCH].rearrange("p (c a n) -> p c a n", c=GCH, a=2)
    SQ = U[:, 0:GCH * 128].rearrange("p (c n) -> p c n", c=GCH)
    Hf = k.sbuf('Hf', [128, 2, NKF, GCH], BF16)
    Y = k.sbuf('Y', [128, 2, NKF, GCH], BF16)
    yout = k.sbuf('yout', [GCH, LO], BF16)
    ssum = k.sbuf('ssum', [128, GCH], F32)
    rn = k.sbuf('rn', [128, GCH], F32)
    rnb = k.sbuf('rnb', [128, 8, GCH], F32)
    bb = k.sbuf('bb', [128, 8, GCH], F32)
    tt = [k.sbuf('tt%d' % i, [128, 8, GCH], BF16) for i in range(4)]
    xsb = [k.sbuf('xsb%d' % i, [128, 2, 8, GCH], BF16) for i in range(2)]
    tf = k.sbuf('tf', [128, 8, GCH], F32)
    vxv = T['vx']

    SQY = Y[:].rearrange("p a k c -> p (a k c)")[:, 0:GCH * 128].rearrange("p (c n) -> p c n", c=GCH)

    def build_batch(g, nb):
        c0 = g * GCH
        dq = nb // 4
        ds = (g * 4 + dq) % 2
        if nb % 4 == 0:
            k.dma('sp', ('dec', ds), dec[ds][:], T['dec'][g, :, dq * 32:(dq + 1) * 32, :], writes=[('dec', ds)])
        bk, kbk = bank(nb % 2)
        pf = bk[:, 0:8 * GCH].rearrange("p (j c) -> p j c", j=8)
        for j in range(8):
            n2 = nb * 8 + j
            k.op('pe', lambda e: e.matmul(pf[:, j, :], lhsT=HD[:, n2, :], rhs=w4s[:, c0:c0 + GCH], start=True, stop=True),
                 reads=['HD', 'w4s'], writes=[kbk])
        k.op('dve', lambda e: e.tensor_tensor(out=XH[:, :, nb * 8:(nb + 1) * 8].rearrange("p c n -> p n c"), in0=pf,
                                              in1=dec[ds][:, (nb % 4) * 8:(nb % 4) * 8 + 8, :], op=ALU.mult),
             reads=[kbk, ('dec', ds)], writes=['XH'])

    def build_stats(g):
        k.op('act', lambda e: e.activation(out=SQY, in_=XH[:], func=AF.Square), reads=['XH'], writes=['Y'])
        k.op('dve', lambda e: e.tensor_reduce(out=ssum[:], in_=SQY, axis=AX.X, op=ALU.add), reads=['Y'], writes=['ssum'])

    for nb in range(16):
        build_batch(0, nb)
    build_stats(0)
    for g in range(NG):
        c0 = g * GCH
        def stage1(nrows):
            for cp in range(GCH // 2):
                bk, kbk = bank(2 + cp % 2)
                pa = bk[:, :].rearrange("p (j x) -> p j x", j=2)
                for j in range(2):
                    c = cp * 2 + j
                    k.op('pe', lambda e: e.matmul(pa[:, j, 0:3 * NKF], lhsT=XH[0:nrows, c, :], rhs=fc[0:nrows, :], start=True, stop=True),
                         reads=['XH', 'fc'], writes=[kbk])
                k.op('act', lambda e: e.activation(out=A_sb[:, cp * 2:cp * 2 + 2, :, :].rearrange("p c a k -> p c (a k)"),
                                                   in_=pa[:, :, 0:3 * NKF], func=AF.Copy), reads=[kbk], writes=['U'])

        def stage2(consume):
            nbat = (NKF + 3) // 4
            for kb in range(nbat):
                nk = min(4, NKF - kb * 4)
                bx, kbx = bank(4 + kb % 4)
                xv = bx[:, :].rearrange("p (j c a) -> p j c a", j=4, a=2)
                for j in range(nk):
                    k1 = kb * 4 + j
                    k.op('pe', lambda e: e.matmul(xv[:, j, :, :], lhsT=gk[:, k1, 0, :], rhs=A_sb[:, :, 1:3, k1], start=True, stop=False),
                         reads=['gk', 'U'], writes=[kbx])
                    k.op('pe', lambda e: e.matmul(xv[:, j, :, :], lhsT=gk[:, k1, 1, :], rhs=A_sb[:, :, 0:2, k1], start=False, stop=True),
                         reads=['gk', 'U'], writes=[kbx])
                consume(kb, nk, xv[:, :, :, 0], kbx, xv[:, :, :, 1], kbx)

        stage1(128)
        bk, kbk = bank(1)
        k.op('pe', lambda e: e.matmul(bk[:, 0:GCH], lhsT=ones[:], rhs=ssum[:], start=True, stop=True), reads=['ones', 'ssum'], writes=[kbk])
        k.op('act', lambda e: e.activation(out=rn[:], in_=bk[:, 0:GCH], func=AF.Sqrt, bias=1e-6, scale=1.0), reads=[kbk], writes=['rn'])
        k.op('dve', lambda e: e.reciprocal(out=rn[:], in_=rn[:]), reads=['rn'], writes=['rn'])
        for j in range(8):
            k.op('dve', lambda e: e.tensor_copy(out=rnb[:, j, :], in_=rn[:]), reads=['rn'], writes=['rnb'])
            k.op('pool', lambda e: e.tensor_copy(out=bb[:, j, :], in_=hbias[:, c0:c0 + GCH]), reads=['hbias'], writes=['bb'])


        def filt_consume(kb, nk, xr, kbr, xi, kbi):
            ks = slice(kb * 4, kb * 4 + nk)
            k.op('dve', lambda e: e.tensor_tensor(out=tf[:, 0:nk, :], in0=xr[:, 0:nk, :], in1=rnb[:, 0:nk, :], op=ALU.mult),
                 reads=[kbr, 'rnb'], writes=['tf'])
            k.op('pool', lambda e: e.tensor_tensor(out=Hf[:, 0, ks, :], in0=tf[:, 0:nk, :], in1=bb[:, 0:nk, :], op=ALU.add),
                 reads=['tf', 'bb'], writes=['Hf'])
            k.op('dve', lambda e: e.tensor_tensor(out=Hf[:, 1, ks, :], in0=xi[:, 0:nk, :], in1=rnb[:, 0:nk, :], op=ALU.mult),
                 reads=[kbi, 'rnb'], writes=['Hf'])
        stage2(filt_consume)

        k.dma('sp', 'xh', XH[0:64, :, :], vxv[c0:c0 + GCH, 1:1 + L].rearrange("c (a n) -> a c n", n=128), reads=[], writes=['XH'])
        stage1(64)

        def data_consume(kb, nk, xr, kbr, xi, kbi):
            ks = slice(kb * 4, kb * 4 + nk)
            hr = Hf[:, 0, ks, :]
            hi = Hf[:, 1, ks, :]
            q = kb % 2
            k.op('act', lambda e: e.activation(out=xsb[q][:, 0, 0:nk, :], in_=xr[:, 0:nk, :], func=AF.Copy), reads=[kbr], writes=[('xsb', q, 0)])
            k.op('act', lambda e: e.activation(out=xsb[q][:, 1, 0:nk, :], in_=xi[:, 0:nk, :], func=AF.Copy), reads=[kbi], writes=[('xsb', q, 1)])
            xrb = xsb[q][:, 0, 0:nk, :]
            xib = xsb[q][:, 1, 0:nk, :]
            k.op('dve', lambda e: e.tensor_tensor(out=tt[0][:, 0:nk, :], in0=xrb, in1=hr, op=ALU.mult), reads=[('xsb', q, 0), 'Hf'], writes=[('tt', 0)])
            k.op('dve', lambda e: e.tensor_tensor(out=tt[1][:, 0:nk, :], in0=xib, in1=hi, op=ALU.mult), reads=[('xsb', q, 1), 'Hf'], writes=[('tt', 1)])
            k.op('dve', lambda e: e.tensor_tensor(out=tt[2][:, 0:nk, :], in0=xrb, in1=hi, op=ALU.mult), reads=[('xsb', q, 0), 'Hf'], writes=[('tt', 2)])
            k.op('dve', lambda e: e.tensor_tensor(out=tt[3][:, 0:nk, :], in0=xib, in1=hr, op=ALU.mult), reads=[('xsb', q, 1), 'Hf'], writes=[('tt', 3)])
            k.op('dve', lambda e: e.tensor_tensor(out=Y[:, 0, ks, :], in0=tt[0][:, 0:nk, :], in1=tt[1][:, 0:nk, :],
                                                  op=ALU.subtract), reads=[('tt', 0), ('tt', 1)], writes=['Y'])
            k.op('dve', lambda e: e.tensor_tensor(out=Y[:, 1, ks, :], in0=tt[2][:, 0:nk, :], in1=tt[3][:, 0:nk, :],
                                                  op=ALU.add), reads=[('tt', 2), ('tt', 3)], writes=['Y'])
        stage2(data_consume)

        for cp in range(GCH // 2):
            bk, kbk = bank(cp % 2)
            pd = bk[:, :].rearrange("p (j x) -> p j x", j=2)
            for j in range(2):
                c = cp * 2 + j
                k.op('pe', lambda e: e.matmul(pd[0:NKF, j, :], lhsT=Y[:, 0, :, c], rhs=cc1[:], start=True, stop=False),
                     reads=['Y', 'cc1'], writes=[kbk])
                k.op('pe', lambda e: e.matmul(pd[0:NKF, j, :], lhsT=Y[:, 1, :, c], rhs=cc2[:], start=False, stop=True),
                     reads=['Y', 'cc2'], writes=[kbk])
            k.op('act', lambda e: e.activation(out=D_sb[0:NKF, cp * 2:cp * 2 + 2, :, :].rearrange("p c a n -> p c (a n)"),
                                               in_=pd[0:NKF, :, :], func=AF.Copy), reads=[kbk], writes=['U'])
        for nb in range(8):
            bk, kbk = bank(2 + nb % 2)
            py = bk[:, :].rearrange("p (j a) -> p j a", j=16)
            for j in range(16):
                n2 = nb * 16 + j
                k.op('pe', lambda e: e.matmul(py[0:GCH, j, :], lhsT=D_sb[0:NKF, :, 0, n2], rhs=mre[0:NKF, n2, :], start=True, stop=False),
                     reads=['U', 'mre'], writes=[kbk])
                k.op('pe', lambda e: e.matmul(py[0:GCH, j, :], lhsT=D_sb[0:NKF, :, 1, n2], rhs=mim[0:NKF, n2, :], start=False, stop=True),
                     reads=['U', 'mim'], writes=[kbk])
            k.op('act', lambda e: e.activation(out=yout[:, :].rearrange("p (a n) -> p n a", n=128)[:, nb * 16:(nb + 1) * 16, :], in_=py[0:GCH, :, :], func=AF.Copy),
                 reads=[kbk], writes=['yout'])
            if g + 1 < NG:
                build_batch(g + 1, 2 * nb)
                build_batch(g + 1, 2 * nb + 1)
        if g + 1 < NG:
            build_stats(g + 1)
        k.dma('sp', 'yco', T['yc'][c0:c0 + GCH, :], yout[:], reads=['yout'], writes=['yc_d'])
    k.end()


PHASES.update(phase3=phase3)


def phase4(k, T):
    k.begin()
    identf = k.sbuf('identf', [128, 128], F32)
    ident = k.sbuf('ident', [128, 128], BF16)
    s1p = k.sbuf('s1p', [128, 8], F32)
    sh1p = k.sbuf('sh1p', [128, 8], F32)
    cs1p = k.sbuf('cs1p', [128, 8], F32)
    csh1p = k.sbuf('csh1p', [128, 8], F32)
    qr = k.sbuf('qr', [128, 4, LO], BF16)
    kr = k.sbuf('kr', [128, NEXT], BF16)
    vt = k.sbuf('vt', [128, 34, 2, 65], BF16)
    kctx = k.sbuf('kctx', [128, 256], BF16)
    vctx = k.sbuf('vctx', [128, 2, 2, 65], BF16)
    xt = [k.sbuf('xt%d' % i, [128, D], F32) for i in range(2)]
    xn = [k.sbuf('xn%d' % i, [128, D], BF16) for i in range(2)]
    st = [k.sbuf('st%d' % i, [128, 2, 6], F32) for i in range(2)]
    mv = [k.sbuf('mv%d' % i, [128, 2], F32) for i in range(2)]
    rstd = [k.sbuf('rstd%d' % i, [128, 1], F32) for i in range(2)]
    banks = [k.psum('bk%d' % i, [128, 512], F32) for i in range(6)]
    pTb = [k.psum('pT%d' % i, [128, 8, 128], BF16) for i in range(2)]

    def bank(i):
        return banks[i], ('bk', i)

    k.dma('sp', 'identf', identf[:], T['ident'], writes=['identf'])
    k.op('act', lambda e: e.activation(out=ident[:], in_=identf[:], func=AF.Copy), reads=['identf'], writes=['ident'])
    load_pvec(k, sh1p[:], T['modv'][0, 0:D], 'sh1p')
    load_pvec(k, s1p[:], T['modv'][0, D:2 * D], 's1p')
    load_pvec(k, csh1p[:], T['modv'][1, 0:D], 'csh1p')
    load_pvec(k, cs1p[:], T['modv'][1, D:2 * D], 'cs1p')
    k.op('dve', lambda e: e.tensor_scalar(out=s1p[:], in0=s1p[:], scalar1=1.0, scalar2=None, op0=ALU.add), reads=['s1p'], writes=['s1p'])
    k.op('dve', lambda e: e.tensor_scalar(out=cs1p[:], in0=cs1p[:], scalar1=1.0, scalar2=None, op0=ALU.add), reads=['cs1p'], writes=['cs1p'])
    k.op('pool', lambda e: e.memset(vt[:].rearrange('p a b c -> p (a b c)'), 1.0), writes=['vt'])
    k.op('pool', lambda e: e.memset(vctx[:].rearrange('p a b c -> p (a b c)'), 1.0), writes=['vctx'])

    tcount = [0]

    def hT_a(src_rows):
        sl = tcount[0] % 2
        tcount[0] += 1
        k.dma('sp', ('xt', sl), xt[sl][:], src_rows, writes=[('xt', sl)])
        ln_tile(k, xt[sl], xn[sl], st[sl], mv[sl], rstd[sl], ('xt', sl), sl)
        return sl

    def hT_b(sl):
        for kc in range(8):
            k.op('pe', lambda e: e.transpose(out=pTb[sl][:, kc, :], in_=xn[sl][:, kc * 128:(kc + 1) * 128], identity=ident[:]),
                 reads=[('xn', sl), 'ident'], writes=[('pT', sl)])

    def hT_c(sl, dst, scp, shp, kdst):
        for kc in range(8):
            k.op('act', lambda e: e.activation(out=dst[:, kc, :], in_=pTb[sl][:, kc, :], func=AF.Identity,
                                               scale=scp[:, kc:kc + 1], bias=shp[:, kc:kc + 1]),
                 reads=[('pT', sl), 's1p', 'sh1p', 'cs1p', 'csh1p'], writes=[kdst])

    def make_hT(src_rows, dst, scp, shp, kdst):
        sl = hT_a(src_rows)
        hT_b(sl)
        hT_c(sl, dst, scp, shp, kdst)

    outer = k.pes
    k.pes = ExitStack()
    k.pes.__enter__()
    NW = 1920
    wB = k.sbuf('wB', [128, 8, NW], BF16)
    wG = k.sbuf('wG', [128, 8, 2048], BF16)
    wst = [k.sbuf('wst%d' % i, [128, 8, 128], F32) for i in range(3)]
    cw = k.sbuf('cw', [128, 12, 4], F32)
    hT = [k.sbuf('hT%d' % i, [128, 8, 512], BF16) for i in range(2)]
    ub = [k.sbuf('ub%d' % i, [128, 4, 514], F32) for i in range(2)]
    tc = k.sbuf('tc', [128, 4, 512], F32)
    ycs = [k.sbuf('ycs%d' % i, [128, 4, 512], BF16) for i in range(2)]
    gsb = k.sbuf('gsb', [128, 8, 512], BF16)
    rc = k.sbuf('rc', [128, 512], F32)
    rs = k.sbuf('rs', [128, 512], F32)
    t1 = k.sbuf('t1', [128, 512], F32)
    t2 = k.sbuf('t2', [128, 512], F32)
    k.dma('sp', 'cw', cw[:], T['cw'], writes=['cw'])
    hmk = k.sbuf('hmk', [128, 2], F32)
    k.dma('sp', 'hmk', hmk[:], T['hmask'], writes=['hmk'])
    wn_ = [0]

    def stage_cast(dst, col0):
        i = wn_[0] % 3
        wn_[0] += 1
        k.dma('sp', ('wst', i), wst[i][:], T['w_in_p'][:, col0:col0 + 128].rearrange("(kc p) n -> p kc n", p=128), writes=[('wst', i)])
        if i % 2 == 0:
            k.op('act', lambda e: e.activation(out=dst, in_=wst[i][:], func=AF.Copy), reads=[('wst', i)], writes=['wB', 'wG'])
        else:
            k.op('dve', lambda e: e.tensor_copy(out=dst, in_=wst[i][:]), reads=[('wst', i)], writes=['wB', 'wG'])
    for j0 in (1536, 1664, 1792, 0, 128, 256, 384, 512, 640, 768, 896, 1024, 1152, 1280, 1408):
        stage_cast(wB[:, :, j0:j0 + 128], C_X0 + j0)
    for j0 in range(0, 2048, 128):
        stage_cast(wG[:, :, j0:j0 + 128], C_G + j0)
    OX0, OQ, OQS, OK_, OKS, OV = 0, 512, 1024, 1536, 1664, 1792

    hc = hT[0]
    for j in range(2):
        make_hT(T['ctx'][j * 128:(j + 1) * 128, :], hc[:, :, j * 128:(j + 1) * 128], cs1p, csh1p, ('hT', 0))
    bk, kbk = bank(0)
    for kc in range(8):
        k.op('pe', lambda e: e.matmul(bk[:, 0:256], lhsT=wB[:, kc, OK_:OK_ + 128], rhs=hc[:, kc, 0:256], start=(kc == 0), stop=(kc == 7)),
             reads=['wB', ('hT', 0)], writes=[kbk])
    k.op('act', lambda e: e.activation(out=kctx[:], in_=bk[:, 0:256], func=AF.Copy), reads=[kbk], writes=['kctx'])
    for j in range(2):
        bk, kbk = bank(1 + j)
        for kc in range(8):
            k.op('pe', lambda e: e.matmul(bk[:, 0:128], lhsT=hc[:, kc, j * 128:(j + 1) * 128], rhs=wB[:, kc, OV:OV + 128], start=(kc == 0), stop=(kc == 7)),
                 reads=['wB', ('hT', 0)], writes=[kbk])
        k.op('act', lambda e: e.activation(out=vctx[:, j, :, 0:64], in_=bk[:, 0:128].rearrange("p (h d) -> p h d", h=2), func=AF.Copy),
             reads=[kbk], writes=['vctx'])

    k.op('pool', lambda e: e.memset(ub[1][:, :, 0:2], 0.0), writes=[('ub', 1, c_) for c_ in range(4)])
    ycv = T['yc'].rearrange("(cc p) t -> p cc t", p=128)
    gtv = T['gt'].rearrange("(cc p) t -> p cc t", p=128)
    bi = [0]

    def nbank():
        bi[0] = (bi[0] + 1) % 6
        return bank(bi[0])

    for ci in range(9):
        s = (ci + 1) % 2
        W = 512 if ci < 8 else 256
        e0 = 4 * ci
        if ci == 0:
            for j in range(W // 128):
                make_hT(T['xext'][(e0 + j) * 128:(e0 + j + 1) * 128, :], hT[s][:, :, j * 128:(j + 1) * 128], s1p, sh1p, ('hT', s))
        k.dma('sp', 'rc', rc[:, 0:W], T['ropec'][:, ci * 512:ci * 512 + W], writes=['rc'])
        k.dma('sp', 'rs', rs[:, 0:W], T['ropes'][:, ci * 512:ci * 512 + W], writes=['rs'])

        def proj(col0, bk, kbk):
            for kc in range(8):
                k.op('pe', lambda e: e.matmul(bk[:, 0:W], lhsT=wB[:, kc, col0:col0 + 128], rhs=hT[s][:, kc, 0:W], start=(kc == 0), stop=(kc == 7)),
                     reads=['wB', ('hT', s)], writes=[kbk])

        o_lo = max(0, 512 * ci - 129)
        o_hi = min(LO, 512 * ci + W - 129)
        j_lo = o_lo - (512 * ci - 129)
        j_hi = o_hi - (512 * ci - 129)
        k.dma('sp', ('ycs', s), ycs[s][:, :, 0:o_hi - o_lo], ycv[:, :, o_lo:o_hi], reads=['yc_d'], writes=[('ycs', s)])
        for cc in range(4):
            bk, kbk = nbank()
            proj(OX0 + cc * 128, bk, kbk)
            k.op('act', lambda e: e.activation(out=ub[s][:, cc, 2:2 + W], in_=bk[:, 0:W], func=AF.Copy), reads=[kbk], writes=[('ub', s, cc)])
            if ci == 0:
                k.op('dve', lambda e: e.tensor_scalar(out=ub[s][:, cc, 2:130], in0=ub[s][:, cc, 2:130], scalar1=hmk[:, 0:1], scalar2=None, op0=ALU.mult),
                     reads=[('ub', s, cc), 'hmk'], writes=[('ub', s, cc)])
            if ci == 8:
                k.op('dve', lambda e: e.tensor_scalar(out=ub[s][:, cc, 130:258], in0=ub[s][:, cc, 130:258], scalar1=hmk[:, 1:2], scalar2=None, op0=ALU.mult),
                     reads=[('ub', s, cc), 'hmk'], writes=[('ub', s, cc)])
            k.op('act', lambda e: e.activation(out=tc[:, cc, 0:W], in_=ub[s][:, cc, 1:1 + W], func=AF.Identity,
                                               scale=cw[:, 8 + cc, 1:2], bias=cw[:, 8 + cc, 3:4]), reads=[('ub', s, cc), 'cw'], writes=[('tc', cc)])
            k.op('dve', lambda e: e.scalar_tensor_tensor(out=tc[:, cc, 0:W], in0=ub[s][:, cc, 0:W], scalar=cw[:, 8 + cc, 0:1], in1=tc[:, cc, 0:W],
                                                         op0=ALU.mult, op1=ALU.add), reads=[('ub', s, cc), 'cw', ('tc', cc)], writes=[('tc', cc)])
            k.op('dve', lambda e: e.scalar_tensor_tensor(out=tc[:, cc, 0:W], in0=ub[s][:, cc, 2:2 + W], scalar=cw[:, 8 + cc, 2:3], in1=tc[:, cc, 0:W],
                                                         op0=ALU.mult, op1=ALU.add), reads=[('ub', s, cc), 'cw', ('tc', cc)], writes=[('tc', cc)])
        k.op('pool', lambda e: e.tensor_tensor(out=ycs[s][:, :, 0:o_hi - o_lo], in0=ycs[s][:, :, 0:o_hi - o_lo], in1=tc[:, :, j_lo:j_hi], op=ALU.mult),
             reads=[('ycs', s)] + [('tc', c_) for c_ in range(4)], writes=[('ycs', s)])
        k.dma('sp', ('yho', s), ycv[:, :, o_lo:o_hi], ycs[s][:, :, 0:o_hi - o_lo], reads=[('ycs', s)], writes=['yc_d'])
        if ci < 8:
            k.op('pool', lambda e: e.tensor_copy(out=ub[1 - s][:, :, 0:2], in_=ub[s][:, :, 512:514]), reads=[('ub', s, c_) for c_ in range(4)], writes=[('ub', 1 - s, c_) for c_ in range(4)])

        def rope(col_a, col_b, dst, kdst, w_lo, w_hi):
            ba, kba = nbank()
            proj(col_a, ba, kba)
            bb_, kbb = nbank()
            proj(col_b, bb_, kbb)
            k.op('dve', lambda e: e.tensor_tensor(out=t1[:, 0:W], in0=ba[:, 0:W], in1=rc[:, 0:W], op=ALU.mult), reads=[kba, 'rc'], writes=['t1'])
            k.op('dve', lambda e: e.tensor_tensor(out=t2[:, 0:W], in0=bb_[:, 0:W], in1=rs[:, 0:W], op=ALU.mult), reads=[kbb, 'rs'], writes=['t2'])
            k.op('pool', lambda e: e.tensor_tensor(out=dst, in0=t1[:, w_lo:w_hi], in1=t2[:, w_lo:w_hi], op=ALU.add), reads=['t1', 't2'], writes=[kdst])

        q_lo = max(0, 512 * ci - 128)
        q_hi = min(LO, 512 * ci + W - 128)
        w_lo = q_lo - (512 * ci - 128)
        w_hi = q_hi - (512 * ci - 128)
        for cc in range(4):
            rope(OQ + cc * 128, OQS + cc * 128, qr[:, cc, q_lo:q_hi], 'qr', w_lo, w_hi)
        rope(OK_, OKS, kr[:, ci * 512:ci * 512 + W], 'kr', 0, W)
        for j in range(W // 128):
            bk, kbk = nbank()
            for kc in range(8):
                k.op('pe', lambda e: e.matmul(bk[:, 0:128], lhsT=hT[s][:, kc, j * 128:(j + 1) * 128], rhs=wB[:, kc, OV:OV + 128], start=(kc == 0), stop=(kc == 7)),
                     reads=['wB', ('hT', s)], writes=[kbk])
            k.op('act', lambda e: e.activation(out=vt[:, e0 + j, :, 0:64], in_=bk[:, 0:128].rearrange("p (h d) -> p h d", h=2), func=AF.Copy),
                 reads=[kbk], writes=['vt'])
        Wn = 0 if ci == 8 else (512 if ci + 1 < 8 else 256)
        for hf in range(2):
            for cc in range(8):
                u = hf * 8 + cc
                pj = u // 4 if (u % 4 == 0 and (u // 4) * 128 < Wn) else None
                if pj is not None:
                    slp = hT_a(T['xext'][(e0 + 4 + pj) * 128:(e0 + 5 + pj) * 128, :])
                bk, kbk = nbank()
                for kc in range(8):
                    k.op('pe', lambda e: e.matmul(bk[:, 0:W], lhsT=wG[:, kc, (hf * 8 + cc) * 128:(hf * 8 + cc + 1) * 128], rhs=hT[s][:, kc, 0:W],
                                                  start=(kc == 0), stop=(kc == 7)), reads=['wG', ('hT', s)], writes=[kbk])
                if pj is not None:
                    hT_b(slp)
                k.op('act', lambda e: e.activation(out=gsb[:, cc, 0:W], in_=bk[:, 0:W], func=AF.Sigmoid), reads=[kbk], writes=['gsb'])
                if pj is not None:
                    hT_c(slp, hT[1 - s][:, :, pj * 128:(pj + 1) * 128], s1p, sh1p, ('hT', 1 - s))
            k.dma('sp', 'gto', gtv[:, hf * 8:(hf + 1) * 8, q_lo:q_hi], gsb[:, :, w_lo:w_hi], reads=['gsb'], writes=['gt_d'])
    if DEBUG:
        k.dma('sp', 'dq1', T['dbg_qr'], qr[:].rearrange("p a b -> p (a b)"), reads=['qr'], writes=['dq1'])
        k.dma('sp', 'dq2', T['dbg_kr'], kr[:], reads=['kr'], writes=['dq2'])
        k.dma('sp', 'dq3', T['dbg_vt'], vt[:].rearrange("p a b c -> p (a b c)"), reads=['vt'], writes=['dq3'])
        k.dma('sp', 'dq4', T['dbg_kc'], kctx[:], reads=['kctx'], writes=['dq4'])
        k.dma('sp', 'dq5', T['dbg_vc'], vctx[:].rearrange("p a b c -> p (a b c)"), reads=['vctx'], writes=['dq5'])
    k.barrier()
    k.pes.__exit__(None, None, None)
    k.pes = outer

    wbh = k.sbuf('wbh', [128, 4, D], BF16)
    wba = k.sbuf('wba', [128, 4, D], BF16)
    wo = k.sbuf('wo', [128, 8, D], BF16)
    wst2 = [k.sbuf('wst2_%d' % i, [128, 4, 256], F32) for i in range(2)]
    w2n = [0]
    msk = k.sbuf('msk', [128, 4, 512], BF16)
    mskf = k.sbuf('mskf', [128, 4, 512], F32)
    esk = k.sbuf('esk', [128, 8], F32)
    g1b = k.sbuf('g1b', [128, D], F32)
    lng = k.sbuf('lng', [128, D], F32)
    lnb = k.sbuf('lnb', [128, D], F32)
    E = [k.sbuf('E%d' % i, [128, 512], BF16) for i in range(6)]
    osb = k.sbuf('osb', [128, 8, 64], BF16)
    den = k.sbuf('den', [128, 4], F32)
    yat = k.sbuf('yat', [128, 4, 512], BF16)
    yh = [k.sbuf('yh%d' % i, [128, 4, 512], BF16) for i in range(2)]
    gts = [k.sbuf('gts%d' % i, [128, 16, 512], BF16) for i in range(2)]
    mT = k.sbuf('mT', [128, 8, 512], BF16)
    m1 = k.sbuf('m1', [128, 512], F32)
    m2 = k.sbuf('m2', [128, 512], F32)
    zt = k.sbuf('zt', [128, D], F32)
    zn = k.sbuf('zn', [128, D], F32)

    def load_w(dst, src, nk):
        for kc2 in range(0, nk, 4):
            for hf in range(4):
                i2 = w2n[0] % 2
                w2n[0] += 1
                k.dma('sp', ('wst2', i2), wst2[i2][:], src[kc2 * 128:(kc2 + 4) * 128, hf * 256:(hf + 1) * 256].rearrange("(kc p) n -> p kc n", p=128), writes=[('wst2', i2)])
                if i2 == 0:
                    k.op('act', lambda e: e.activation(out=dst[:, kc2:kc2 + 4, hf * 256:(hf + 1) * 256], in_=wst2[i2][:], func=AF.Copy), reads=[('wst2', i2)], writes=['w2'])
                else:
                    k.op('dve', lambda e: e.tensor_copy(out=dst[:, kc2:kc2 + 4, hf * 256:(hf + 1) * 256], in_=wst2[i2][:]), reads=[('wst2', i2)], writes=['w2'])
    deferred = [lambda: load_w(wbh, T['w_bh'], 4), lambda: load_w(wba, T['w_ba'], 4), lambda: load_w(wo, T['w_o'], 8)]
    k.dma('sp', 'mskf', mskf[:], T['masks'], writes=['mskf'])
    k.op('act', lambda e: e.activation(out=msk[:], in_=mskf[:], func=AF.Copy), reads=['mskf'], writes=['msk'])
    k.dma('sp', 'esk', esk[:], T['sinks'], writes=['esk'])
    k.op('act', lambda e: e.activation(out=esk[:], in_=esk[:], func=AF.Exp), reads=['esk'], writes=['esk'])
    k.dma('sp', 'g1b', g1b[:], T['modv'][0:1, 2 * D:3 * D].broadcast_to([128, D]), writes=['g1b'])
    k.dma('sp', 'lng', lng[:], T['ln1'][0:1, :].broadcast_to([128, D]), writes=['lng'])
    k.dma('sp', 'lnb', lnb[:], T['ln1'][1:2, :].broadcast_to([128, D]), writes=['lnb'])

    ei = [0]
    osbs = [osb, k.sbuf('osb1', [128, 8, 64], BF16)]

    def attn_tile(oc, tj):
        t = oc * 4 + tj
        e = t + 1
        ob, kob = osbs[t % 2], ('osb', t % 2)
        for G in range(2):
            ps = slice(64 * G, 64 * G + 64)
            q4 = qr[ps, :, t * 128:(t + 1) * 128]
            blocks = [('w', e - 1, 2 if t == 0 else 0), ('w', e, None), ('w', e + 1, 3 if t == 31 else 1), ('c', 0, None), ('c', 1, None)]
            Es = []
            for (kind, idx, mi) in blocks:
                bk, kbk = bank(ei[0] % 3)
                Eb, kE = E[ei[0] % 6], ('E', ei[0] % 6)
                ei[0] += 1
                kk_ = kr[ps, idx * 128:(idx + 1) * 128] if kind == 'w' else kctx[ps, idx * 128:(idx + 1) * 128]
                k.op('pe', lambda e_: e_.matmul(bk[:, :], lhsT=kk_, rhs=q4, start=True, stop=(mi is None)),
                     reads=['kr', 'kctx', 'qr'], writes=[kbk])
                if mi is not None:
                    k.op('pe', lambda e_: e_.matmul(bk[:, :], lhsT=ident[:], rhs=msk[:, mi, :], start=False, stop=True),
                         reads=['ident', 'msk'], writes=[kbk])
                k.op('act', lambda e_: e_.activation(out=Eb[:], in_=bk[:, :], func=AF.Exp, scale=0.125), reads=[kbk], writes=[kE])
                Es.append((Eb, kE, kind, idx))
            bo, kbo = bank(3 + G)
            po = bo[:, 0:4 * 80].rearrange("p (j x) -> p j x", j=4)
            for j in range(4):
                for bi_, (Eb, kE, kind, idx) in enumerate(Es):
                    vv = vt[:, idx, G, :] if kind == 'w' else vctx[:, idx, G, :]
                    k.op('pe', lambda e_: e_.matmul(po[:, j, 0:65], lhsT=Eb[:, j * 128:(j + 1) * 128], rhs=vv, start=(bi_ == 0), stop=(bi_ == 4)),
                         reads=[kE, 'vt', 'vctx'], writes=[kbo])
            k.op('dve', lambda e_: e_.tensor_tensor(out=den[:, 4 * G:4 * G + 4], in0=po[:, :, 64], in1=esk[:, 4 * G:4 * G + 4], op=ALU.add),
                 reads=[kbo, 'esk'], writes=[('den', G)])
            k.op('dve', lambda e_: e_.reciprocal(out=den[:, 4 * G:4 * G + 4], in_=den[:, 4 * G:4 * G + 4]), reads=[('den', G)], writes=[('den', G)])
            for j in range(4):
                k.op('act', lambda e_: e_.activation(out=ob[:, 4 * G + j, :], in_=po[:, j, 0:64], func=AF.Identity, scale=den[:, 4 * G + j:4 * G + j + 1]),
                     reads=[kbo, ('den', G)], writes=[kob])

        def fin():
            for kc in range(4):
                k.op('pe', lambda e_: e_.transpose(out=pTb[0][:, kc, :], in_=ob[:, 2 * kc:2 * kc + 2, :].rearrange("p h d -> p (h d)"), identity=ident[:]),
                     reads=[kob, 'ident'], writes=[('pT', 0)])
            k.op('dve', lambda e_: e_.tensor_copy(out=yat[:, :, tj * 128:(tj + 1) * 128], in_=pTb[0][:, 0:4, :]), reads=[('pT', 0)], writes=['yat'])
        return fin

    def branch(oc):
        s = oc % 2
        for dc in range(8):
            bh, kbh = bank(5)
            ba, kba = bank(4 - (dc % 2))
            for kc in range(4):
                k.op('pe', lambda e_: e_.matmul(bh[:, :], lhsT=wbh[:, kc, dc * 128:(dc + 1) * 128], rhs=yh[s][:, kc, :], start=(kc == 0), stop=(kc == 3)),
                     reads=['w2', ('yh', s)], writes=[kbh])
            for kc in range(4):
                k.op('pe', lambda e_: e_.matmul(ba[:, :], lhsT=wba[:, kc, dc * 128:(dc + 1) * 128], rhs=yat[:, kc, :], start=(kc == 0), stop=(kc == 3)),
                     reads=['w2', 'yat'], writes=[kba])
            k.op('dve', lambda e_: e_.tensor_tensor(out=m1[:], in0=bh[:, :], in1=gts[s][:, dc, :], op=ALU.mult), reads=[kbh, ('gts', s)], writes=['m1'])
            k.op('dve', lambda e_: e_.tensor_tensor(out=m2[:], in0=ba[:, :], in1=gts[s][:, 8 + dc, :], op=ALU.mult), reads=[kba, ('gts', s)], writes=['m2'])
            k.op('dve', lambda e_: e_.tensor_tensor(out=mT[oc % 2][:, dc, :], in0=m1[:], in1=m2[:], op=ALU.add), reads=['m1', 'm2'], writes=['mT'])

    def outproj_tile(oc, tj):
        t = oc * 4 + tj
        sl = t % 2
        k.dma('sp', ('xt', sl), xt[sl][:], T['xext'][(t + 1) * 128:(t + 2) * 128, :], writes=[('xt', sl)])
        for hf in range(2):
            bk, kbk = bank(5 if hf == 0 else 4)
            for kc in range(8):
                k.op('pe', lambda e_: e_.matmul(bk[:, :], lhsT=mT[oc % 2][:, kc, tj * 128:(tj + 1) * 128], rhs=wo[:, kc, hf * 512:(hf + 1) * 512],
                                                start=(kc == 0), stop=(kc == 7)), reads=['mT', 'w2'], writes=[kbk])
            k.op('dve', lambda e_: e_.tensor_tensor(out=zt[:, hf * 512:(hf + 1) * 512], in0=bk[:, :], in1=g1b[:, hf * 512:(hf + 1) * 512], op=ALU.mult),
                 reads=[kbk, 'g1b'], writes=['zt'])
        k.op('dve', lambda e_: e_.scalar_tensor_tensor(out=zt[:], in0=xt[sl][:], scalar=ALPHA, in1=zt[:], op0=ALU.mult, op1=ALU.add),
             reads=[('xt', sl), 'zt'], writes=['zt'])
        ln_tile(k, zt, zns[sl], st[sl], mv[sl], rstd[sl], 'zt', sl, kout=('zn', sl))
        k.op('dve', lambda e_: e_.tensor_tensor(out=zns[sl][:], in0=zns[sl][:], in1=lng[:], op=ALU.mult), reads=[('zn', sl), 'lng'], writes=[('zn', sl)])
        k.op('dve', lambda e_: e_.tensor_tensor(out=zns[sl][:], in0=zns[sl][:], in1=lnb[:], op=ALU.add), reads=[('zn', sl), 'lnb'], writes=[('zn', sl)])
        k.dma('sp', ('x1o', sl), T['x1'][t * 128:(t + 1) * 128, :], zns[sl][:], reads=[('zn', sl)], writes=['x1_d'])

    mT = [mT, mT]
    zns = [zn, k.sbuf('zn1', [128, D], F32)]
    den = k.sbuf('den8', [128, 8], F32)
    pending = []
    for oc in range(8):
        s = oc % 2
        k.dma('sp', ('yh', s), yh[s][:], ycv[:, :, oc * 512:(oc + 1) * 512], reads=['yc_d'], writes=[('yh', s)])
        k.dma('sp', ('gts', s), gts[s][:], gtv[:, :, oc * 512:(oc + 1) * 512], reads=['gt_d'], writes=[('gts', s)])
        prev_fin = None
        for tj in range(4):
            fin = attn_tile(oc, tj)
            if deferred:
                deferred.pop(0)()
            if prev_fin is not None:
                prev_fin()
            if pending:
                outproj_tile(*pending.pop(0))
            prev_fin = fin
        prev_fin()
        branch(oc)
        pending = [(oc, tj) for tj in range(4)]
    while pending:
        outproj_tile(*pending.pop(0))
    k.end()


PHASES.update(phase4=phase4)


def phase5(k, T):
    k.begin()
    identf = k.sbuf('identf', [128, 128], F32)
    ident = k.sbuf('ident', [128, 128], BF16)
    s2p = k.sbuf('s2p', [128, 8], F32)
    sh2p = k.sbuf('sh2p', [128, 8], F32)
    g2b = k.sbuf('g2b', [128, D], F32)
    lng = k.sbuf('lng', [128, D], F32)
    lnb = k.sbuf('lnb', [128, D], F32)
    bgr = k.sbuf('bgr', [128, 20], F32)
    self_ = k.sbuf('self', [16, 16, 128], F32)
    sel = k.sbuf('sel', [16, 16, 128], BF16)
    wgrf = k.sbuf('wgrf', [128, 8, 20], F32)
    wgr = k.sbuf('wgr', [128, 8, 20], BF16)
    wd = k.sbuf('wd', [128, NE, 2, D], BF16)
    hid = k.sbuf('hid', [128, NE, 2, 512], BF16)
    t2T = [k.sbuf('t2T%d' % i, [128, 8, 512], BF16) for i in range(2)]
    gT = [k.sbuf('gT%d' % i, [16, 512], BF16) for i in range(2)]
    wgb = [k.sbuf('wgb%d' % i, [128, 8, DE], BF16) for i in range(3)]
    wub = [k.sbuf('wub%d' % i, [128, 8, DE], BF16) for i in range(3)]
    xt = [k.sbuf('xt%d' % i, [128, D], F32) for i in range(2)]
    xn = [k.sbuf('xn%d' % i, [128, D], BF16) for i in range(2)]
    st = [k.sbuf('st%d' % i, [128, 2, 6], F32) for i in range(2)]
    mv = [k.sbuf('mv%d' % i, [128, 2], F32) for i in range(2)]
    rstd = [k.sbuf('rstd%d' % i, [128, 1], F32) for i in range(2)]
    xr = [k.sbuf('xr%d' % i, [128, D], F32) for i in range(2)]
    zt = k.sbuf('zt', [128, D], F32)
    zn = [k.sbuf('zn%d' % i, [128, D], F32) for i in range(2)]
    sg = [k.sbuf('sg%d' % i, [128, 512], F32) for i in range(2)]
    tm = [k.sbuf('tm%d' % i, [128, 512], F32) for i in range(2)]
    lg = k.sbuf('lg', [128, 20], F32)
    sm = k.sbuf('sm', [128, 16], F32)
    oh = k.sbuf('oh', [128, 4], F32)
    ig = k.sbuf('ig', [128, 4], F32)
    ig2 = k.sbuf('ig2', [128, 4], F32)
    oh1 = k.sbuf('oh1', [128, 4], F32)
    oh2 = k.sbuf('oh2', [128, 4], F32)
    eg = k.sbuf('eg', [128, 4], F32)
    ws = k.sbuf('ws', [128, 4], F32)
    g16 = k.sbuf('g16', [128, 16], F32)
    banks = [k.psum('bk%d' % i, [128, 512], F32) for i in range(7)]
    pTb = [k.psum('pT%d' % i, [128, 8, 128], BF16) for i in range(1)]

    def bank(i):
        return banks[i], ('bk', i)

    k.dma('sp', 'identf', identf[:], T['ident'], writes=['identf'])
    k.op('act', lambda e: e.activation(out=ident[:], in_=identf[:], func=AF.Copy), reads=['identf'], writes=['ident'])
    load_pvec(k, sh2p[:], T['modv'][0, 3 * D:4 * D], 'sh2p')
    load_pvec(k, s2p[:], T['modv'][0, 4 * D:5 * D], 's2p')
    k.op('dve', lambda e: e.tensor_scalar(out=s2p[:], in0=s2p[:], scalar1=1.0, scalar2=None, op0=ALU.add), reads=['s2p'], writes=['s2p'])
    k.dma('sp', 'g2b', g2b[:], T['modv'][0:1, 5 * D:6 * D].broadcast_to([128, D]), writes=['g2b'])
    k.dma('sp', 'lng', lng[:], T['ln2'][0:1, :].broadcast_to([128, D]), writes=['lng'])
    k.dma('sp', 'lnb', lnb[:], T['ln2'][1:2, :].broadcast_to([128, D]), writes=['lnb'])
    k.dma('sp', 'bgr', bgr[:], T['b_gr'], writes=['bgr'])
    k.dma('sp', 'self', self_[:], T['sel'], writes=['self'])
    k.op('act', lambda e: e.activation(out=sel[:], in_=self_[:], func=AF.Copy), reads=['self'], writes=['sel'])
    k.dma('sp', 'wgrf', wgrf[:], T['w_gr'].rearrange("(kc p) n -> p kc n", p=128), writes=['wgrf'], slow=True)
    k.op('act', lambda e: e.activation(out=wgr[:], in_=wgrf[:], func=AF.Copy), reads=['wgrf'], writes=['wgr'])

    tcount = [0]
    R = ['lg', 'sm', 'oh', 'ig', 'ig2', 'oh1', 'oh2', 'eg', 'ws']

    pst = {}

    def prep_a(sc, j):
        sl = tcount[0] % 2
        tcount[0] += 1
        t = sc * 4 + j
        k.dma('sp', ('xt', sl), xt[sl][:], T['x1'][t * 128:(t + 1) * 128, :], reads=['x1_d'], writes=[('xt', sl)])
        ln_tile(k, xt[sl], xn[sl], st[sl], mv[sl], rstd[sl], ('xt', sl), sl)
        pst[(sc, j)] = sl

    def prep_b(sc, j):
        bf = sc % 2
        sl = pst[(sc, j)]
        for kc in range(8):
            k.op('pe', lambda e: e.transpose(out=pTb[0][:, kc, :], in_=xn[sl][:, kc * 128:(kc + 1) * 128], identity=ident[:]),
                 reads=[('xn', sl), 'ident'], writes=['pT'])
        for kc in range(8):
            k.op('act', lambda e: e.activation(out=t2T[bf][:, kc, j * 128:(j + 1) * 128], in_=pTb[0][:, kc, :], func=AF.Identity,
                                               scale=s2p[:, kc:kc + 1], bias=sh2p[:, kc:kc + 1]),
                 reads=['pT', 's2p', 'sh2p'], writes=[('t2T', bf)])

    def prep_c(sc, j):
        bf = sc % 2
        bk, kbk = bank(6)
        for kc in range(8):
            k.op('pe', lambda e: e.matmul(bk[:, 0:20], lhsT=t2T[bf][:, kc, j * 128:(j + 1) * 128], rhs=wgr[:, kc, :], start=(kc == 0), stop=(kc == 7)),
                 reads=[('t2T', bf), 'wgr'], writes=[kbk])

        def dv(fn, reads=R, writes=R):
            k.op('dve', fn, reads=reads, writes=writes)
        dv(lambda e: e.tensor_tensor(out=lg[:], in0=bk[:, 0:20], in1=bgr[:], op=ALU.add), reads=[kbk, 'bgr'] + R)
        el = lg[:, 4:20].rearrange("p (g e) -> p g e", g=4)
        dv(lambda e: e.tensor_reduce(out=sm[:, 0:1], in_=lg[:, 0:4], axis=AX.X, op=ALU.max))
        dv(lambda e: e.tensor_scalar(out=oh[:], in0=lg[:, 0:4], scalar1=sm[:, 0:1], scalar2=None, op0=ALU.is_equal))
        dv(lambda e: e.tensor_scalar(out=eg[:], in0=lg[:, 0:4], scalar1=sm[:, 0:1], scalar2=None, op0=ALU.subtract))
        k.op('act', lambda e: e.activation(out=eg[:], in_=eg[:], func=AF.Exp), reads=R, writes=R)
        dv(lambda e: e.tensor_reduce(out=sm[:, 1:2], in_=eg[:], axis=AX.X, op=ALU.add))
        dv(lambda e: e.reciprocal(out=sm[:, 1:2], in_=sm[:, 1:2]))
        dv(lambda e: e.tensor_scalar(out=ig[:], in0=el[:, 0, :], scalar1=oh[:, 0:1], scalar2=None, op0=ALU.mult))
        for g in range(1, 4):
            dv(lambda e: e.scalar_tensor_tensor(out=ig[:], in0=el[:, g, :], scalar=oh[:, g:g + 1], in1=ig[:], op0=ALU.mult, op1=ALU.add))
        dv(lambda e: e.tensor_reduce(out=sm[:, 2:3], in_=ig[:], axis=AX.X, op=ALU.max))
        dv(lambda e: e.tensor_scalar(out=oh1[:], in0=ig[:], scalar1=sm[:, 2:3], scalar2=None, op0=ALU.is_equal))
        dv(lambda e: e.scalar_tensor_tensor(out=ig2[:], in0=oh1[:], scalar=-1e30, in1=ig[:], op0=ALU.mult, op1=ALU.add))
        dv(lambda e: e.tensor_reduce(out=sm[:, 3:4], in_=ig2[:], axis=AX.X, op=ALU.max))
        dv(lambda e: e.tensor_scalar(out=oh2[:], in0=ig2[:], scalar1=sm[:, 3:4], scalar2=None, op0=ALU.is_equal))
        dv(lambda e: e.tensor_tensor(out=sm[:, 4:5], in0=sm[:, 3:4], in1=sm[:, 2:3], op=ALU.subtract))
        k.op('act', lambda e: e.activation(out=sm[:, 5:6], in_=sm[:, 4:5], func=AF.Exp), reads=R, writes=R)
        dv(lambda e: e.tensor_scalar(out=sm[:, 6:7], in0=sm[:, 5:6], scalar1=1.0, scalar2=None, op0=ALU.add))
        dv(lambda e: e.reciprocal(out=sm[:, 6:7], in_=sm[:, 6:7]))
        dv(lambda e: e.tensor_tensor(out=sm[:, 7:8], in0=sm[:, 6:7], in1=sm[:, 1:2], op=ALU.mult))
        dv(lambda e: e.tensor_tensor(out=sm[:, 8:9], in0=sm[:, 7:8], in1=sm[:, 5:6], op=ALU.mult))
        dv(lambda e: e.tensor_scalar(out=ws[:], in0=oh1[:], scalar1=sm[:, 7:8], scalar2=None, op0=ALU.mult))
        dv(lambda e: e.scalar_tensor_tensor(out=ws[:], in0=oh2[:], scalar=sm[:, 8:9], in1=ws[:], op0=ALU.mult, op1=ALU.add))
        for g in range(4):
            k.op('dve', lambda e: e.tensor_scalar(out=g16[:, 4 * g:4 * g + 4], in0=ws[:], scalar1=oh[:, g:g + 1], scalar2=None, op0=ALU.mult),
                 reads=R, writes=['g16'])

    def prep_d(sc, j):
        bf = sc % 2
        bk, kbk = bank(6)
        k.op('pe', lambda e: e.transpose(out=bk[0:16, 128:256], in_=g16[:], identity=identf[:]), reads=['g16', 'identf'], writes=[kbk])
        k.op('act', lambda e: e.activation(out=gT[bf][:, j * 128:(j + 1) * 128], in_=bk[0:16, 128:256], func=AF.Copy), reads=[kbk], writes=[('gT', bf)])

    wn = [0]

    def load_expert(e):
        i = wn[0] % 3
        wn[0] += 1
        k.dma('sp', ('wgb', i), wgb[i][:].rearrange("p a b -> p (a b)"), T['wgu'][e, 0], writes=[('wgb', i)])
        k.dma('sp', ('wub', i), wub[i][:].rearrange("p a b -> p (a b)"), T['wgu'][e, 1], writes=[('wub', i)])
        return i

    for j in range(4):
        prep_a(0, j)
        prep_b(0, j)
        prep_c(0, j)
        prep_d(0, j)
    for e in range(NE):
        k.dma('sp', 'wd', wd[:, e, :, :].rearrange("p a b -> p (a b)"), T['wdb'][e], writes=['wd'])
    pend = [load_expert(0)]
    fcn = [0]
    for sc in range(8):
        bf = sc % 2
        for e in range(NE):
            wi = pend.pop(0)
            nxt = (sc * NE + e + 1)
            if nxt < 8 * NE:
                pend.append(load_expert(nxt % NE))
            bw, kbw = bank(4 + e % 2)
            k.op('pe', lambda en: en.matmul(bw[:, :], lhsT=sel[:, e, :], rhs=gT[bf][:, :], start=True, stop=True), reads=['sel', ('gT', bf)], writes=[kbw])
            pj = (e - 1) // 4 if (sc < 7 and e % 4 == 1) else None
            dj = (e - 2) // 4 if (sc < 7 and e % 4 == 2) else None
            if pj is not None:
                prep_a(sc + 1, pj)
            for fc in range(2):
                q = fcn[0] % 2
                fcn[0] += 1
                bg, kbg = bank(q)
                bu, kbu = bank(2 + q)
                for kc in range(8):
                    k.op('pe', lambda en: en.matmul(bg[:, :], lhsT=wgb[wi][:, kc, fc * 128:(fc + 1) * 128], rhs=t2T[bf][:, kc, :], start=(kc == 0), stop=(kc == 7)),
                         reads=[('wgb', wi), ('t2T', bf)], writes=[kbg])
                for kc in range(8):
                    k.op('pe', lambda en: en.matmul(bu[:, :], lhsT=wub[wi][:, kc, fc * 128:(fc + 1) * 128], rhs=t2T[bf][:, kc, :], start=(kc == 0), stop=(kc == 7)),
                         reads=[('wub', wi), ('t2T', bf)], writes=[kbu])
                k.op('act', lambda en: en.activation(out=sg[q][:], in_=bg[:, :], func=AF.Silu), reads=[kbg], writes=[('sg', q)])
                k.op('dve', lambda en: en.tensor_tensor(out=tm[q][:], in0=sg[q][:], in1=bu[:, :], op=ALU.mult), reads=[('sg', q), kbu], writes=[('tm', q)])
                k.op('dve', lambda en: en.tensor_tensor(out=hid[:, e, fc, :], in0=tm[q][:], in1=bw[:, :], op=ALU.mult), reads=[('tm', q), kbw], writes=['hid'])
                if pj is not None and fc == 0:
                    prep_b(sc + 1, pj)
                if pj is not None and fc == 1:
                    prep_c(sc + 1, pj)
            if dj is not None:
                prep_d(sc + 1, dj)
        for j in range(4):
            t = sc * 4 + j
            sl = t % 2
            k.dma('sp', ('xr', sl), xr[sl][:], T['x1'][t * 128:(t + 1) * 128, :], reads=['x1_d'], writes=[('xr', sl)])
            for hf in range(2):
                q = fcn[0] % 2
                fcn[0] += 1
                bk, kbk = bank(2 * (hf % 2) + q) if False else bank(q + 2 * hf)
                n = 0
                for e in range(NE):
                    for fc in range(2):
                        k.op('pe', lambda en: en.matmul(bk[:, :], lhsT=hid[:, e, fc, j * 128:(j + 1) * 128], rhs=wd[:, e, fc, hf * 512:(hf + 1) * 512],
                                                        start=(n == 0), stop=(n == 2 * NE - 1)), reads=['hid', 'wd'], writes=[kbk])
                        n += 1
                k.op('dve', lambda en: en.tensor_tensor(out=zt[:, hf * 512:(hf + 1) * 512], in0=bk[:, :], in1=g2b[:, hf * 512:(hf + 1) * 512], op=ALU.mult),
                     reads=[kbk, 'g2b'], writes=['zt'])
            k.op('dve', lambda en: en.scalar_tensor_tensor(out=zt[:], in0=xr[sl][:], scalar=ALPHA, in1=zt[:], op0=ALU.mult, op1=ALU.add),
                 reads=[('xr', sl), 'zt'], writes=['zt'])
            ln_tile(k, zt, zn[sl], st[sl], mv[sl], rstd[sl], 'zt', sl, kout=('zn', sl))
            k.op('pool', lambda en: en.tensor_tensor(out=zn[sl][:], in0=zn[sl][:], in1=lng[:], op=ALU.mult), reads=[('zn', sl), 'lng'], writes=[('zn', sl)])
            k.op('pool', lambda en: en.tensor_tensor(out=zn[sl][:], in0=zn[sl][:], in1=lnb[:], op=ALU.add), reads=[('zn', sl), 'lnb'], writes=[('zn', sl)])
            k.dma('sp', ('outo', sl), T['out'][t * 128:(t + 1) * 128, :], zn[sl][:], reads=[('zn', sl)], writes=['out_d'])
    k.end()


PHASES.update(phase5=phase5)


_NC_CACHE = {}


def kernel(**inputs):
    maps = _host_inputs(inputs)
    if 'nc' not in _NC_CACHE:
        _NC_CACHE['nc'] = build_nc()
    nc = _NC_CACHE['nc']
    res = run_bass_kernel_spmd(nc, maps, core_ids=list(range(8)))
    out = np.empty((4, L, D), np.float32)
    for core in range(8):
        b, half = core // 2, core % 2
        out[b, half * LO:(half + 1) * LO] = np.asarray(res.results[core]['out'])
    return out
```

```python
import math
from contextlib import ExitStack

import numpy as np
import ml_dtypes

import concourse.bass as bass
import concourse.mybir as mybir
from concourse.bass_utils import run_bass_kernel_spmd

F32 = mybir.dt.float32
BF16 = mybir.dt.bfloat16
I32 = mybir.dt.int32
U32 = mybir.dt.uint32
ALU = mybir.AluOpType
AF = mybir.ActivationFunctionType
AX = mybir.AxisListType

D = 1024
L = 8192
LO = 4096
NB = 64
NEXT = LO + 256
DH = 512
NKF = 65
GCH = 64
NG = DH // GCH
NE = 16
DE = 256
ALPHA = 2.0 ** 0.25
EPS = 1e-5
TWO_PI = 2.0 * math.pi
C_X1V = 0
C_X0 = 1024
C_Q = 1536
C_QS = 2048
C_K = 2560
C_KS = 2688
C_V = 2816
C_G = 2944
NCOL = 4992


class K:
    def __init__(self, nc, es):
        self.nc = nc
        self.es = es
        self.engs = {'pe': nc.tensor, 'act': nc.scalar, 'dve': nc.vector, 'pool': nc.gpsimd, 'sp': nc.sync}
        self.esem = {}
        self.ecnt = {}
        for e in ['pe', 'act', 'dve', 'pool']:
            self.esem[e] = es.enter_context(nc.semaphore('s_' + e))
            self.ecnt[e] = 0
        self.sems = {}
        for e in self.esem:
            self.sems[id(self.esem[e])] = [self.esem[e], 0]
        self.waited = {}
        self.lastw = {}
        self.readers = {}
        self.dsem = {}
        self.pes = None
        self.nsem = 0

    def begin(self):
        self.pes = ExitStack()
        self.pes.__enter__()
        self.phase = getattr(self, 'phase', 0) + 1

    def end(self):
        self.barrier()
        self.lastw.clear()
        self.readers.clear()
        self.pes.__exit__(None, None, None)
        self.pes = None

    def sbuf(self, name, shape, dt):
        return self.pes.enter_context(self.nc.sbuf_tensor('s%d_%s' % (self.phase, name), shape, dt))

    def psum(self, name, shape, dt):
        return self.pes.enter_context(self.nc.psum_tensor('q%d_%s' % (self.phase, name), shape, dt))

    def _wait(self, e, ev):
        if ev is None:
            return
        sem, val = ev
        if e == 'pe' and sem is self.esem['pe']:
            return
        kk = (e, id(sem))
        if self.waited.get(kk, 0) >= val:
            return
        self.engs[e].wait_ge(sem, val)
        self.waited[kk] = val

    def _deps(self, e, reads, writes):
        for r in reads:
            self._wait(e, self.lastw.get(r))
        for w in writes:
            self._wait(e, self.lastw.get(w))
            for ev in self.readers.get(w, {}).values():
                self._wait(e, ev)

    def _commit(self, ev, reads, writes):
        sid = id(ev[0])
        for r in reads:
            self.readers.setdefault(r, {})[sid] = ev
        for w in writes:
            self.lastw[w] = ev
            self.readers[w] = {}

    def op(self, e, fn, reads=(), writes=()):
        if e == 'pool':
            e = 'dve'
        self._deps(e, reads, writes)
        ins = fn(self.engs[e])
        self.ecnt[e] += 1
        ins.then_inc(self.esem[e], 1)
        ev = (self.esem[e], self.ecnt[e])
        self.sems[id(self.esem[e])][1] = self.ecnt[e]
        self._commit(ev, reads, writes)
        return ev

    def dma(self, q, semkey, out, in_, reads=(), writes=(), slow=False):
        if semkey not in self.dsem:
            s = self.es.enter_context(self.nc.semaphore('d%d' % self.nsem))
            self.nsem += 1
            self.dsem[semkey] = [s, 0]
            self.sems[id(s)] = [s, 0]
        self._deps(q, reads, writes)
        s = self.dsem[semkey]
        s[1] += 16
        if slow:
            ins = self.engs[q].dma_start(out=out, in_=in_, allow_slow_non_contiguous=True)
        else:
            ins = self.engs[q].dma_start(out=out, in_=in_)
        ins.then_inc(s[0], 16)
        ev = (s[0], s[1])
        self.sems[id(s[0])][1] = s[1]
        self._commit(ev, reads, writes)
        return ev

    def barrier(self):
        for e in ['sp', 'pe', 'act', 'dve', 'pool']:
            for sem, val in self.sems.values():
                if val > 0:
                    self._wait(e, (sem, val))


_CONST = {}


def _consts():
    if _CONST:
        return _CONST
    f64 = np.float64
    N = 2 * L
    n1 = np.arange(128, dtype=f64)
    k1 = np.arange(NKF, dtype=f64)
    ang = TWO_PI * np.outer(n1, k1) / 128.0
    _CONST['fc'] = np.concatenate([np.sin(ang), np.cos(ang), -np.sin(ang)], axis=1).astype(np.float32)
    n2 = np.arange(128, dtype=f64)
    k2 = np.arange(128, dtype=f64)
    kk = k1[None, :, None] + 128.0 * k2[None, None, :]
    a2 = TWO_PI * n2[:, None, None] * kk / N
    g = np.stack([np.cos(a2), -np.sin(a2)], axis=2)
    _CONST['gk'] = g.astype(ml_dtypes.bfloat16)
    a3 = TWO_PI * np.outer(k2, n2) / 128.0
    _CONST['cc1'] = np.concatenate([np.cos(a3), np.sin(a3)], axis=1).astype(np.float32)
    _CONST['cc2'] = np.concatenate([-np.sin(a3), np.cos(a3)], axis=1).astype(np.float32)
    t = np.linspace(0.0, 1.0, L, dtype=np.float32)[:, None]
    w = (2.0 * math.pi * np.arange(L, dtype=np.float32)[:, None] / L).astype(np.float32)
    bands = np.linspace(1e-4, 15, 16, dtype=np.float32)
    z = np.concatenate([t, np.cos(bands * w), -np.sin(bands * w)], axis=-1).astype(np.float32)
    _CONST['zz'] = np.ascontiguousarray(np.concatenate([z.T, z[::-1].T], axis=0))
    max_decay = math.log(1e-2) / 0.3
    min_decay = math.log(1e-2) / 1.5
    deltas = np.linspace(min_decay, max_decay, DH, dtype=np.float32)
    tt = np.linspace(0.0, 1.0, L, dtype=np.float32)
    pos = np.arange(L).reshape(64, 128)
    posb = np.concatenate([pos, (L - 1) - pos], axis=0)
    dec = np.exp(-tt[posb][:, :, None] * np.abs(deltas)[None, None, :]).astype(np.float32)
    _CONST['dec'] = np.ascontiguousarray(dec.reshape(128, 128, NG, GCH).transpose(2, 0, 1, 3))
    _CONST['ident'] = np.eye(128, dtype=np.float32)
    sel = np.zeros((16, 16, 128), np.float32)
    for e_ in range(16):
        sel[e_, e_, :] = 1.0
    _CONST['sel'] = sel
    return _CONST


def _core_consts(half):
    f64 = np.float64
    N = 2 * L
    t0 = half * LO
    k1 = np.arange(NKF, dtype=f64)
    wk = np.where((k1 == 0) | (k1 == 64), 1.0, 2.0) / N
    n2 = np.arange(128, dtype=f64)
    n1 = np.arange(32, dtype=f64) + t0 // 128
    n = 128.0 * n1[None, None, :] + n2[None, :, None]
    ang = TWO_PI * k1[:, None, None] * n / N
    mre = (wk[:, None, None] * np.cos(ang)).astype(np.float32)
    mim = (-wk[:, None, None] * np.sin(ang)).astype(np.float32)
    tok = np.arange(t0 - 128, t0 + LO + 128)
    tokc = np.clip(tok, 0, L - 1)
    row = (tokc // 64).astype(np.float32)
    col = (tokc % 64).astype(np.float32)
    half_d = 32
    inv_freq = (10000.0 ** (-np.arange(0, half_d, 2, dtype=np.float32) / half_d)).astype(np.float32)
    ang2 = np.concatenate([row[:, None] * inv_freq, col[:, None] * inv_freq], axis=-1)
    cs, sn = np.cos(ang2), np.sin(ang2)
    d = np.arange(64)
    C = cs[:, d // 2]
    S = sn[:, d // 2] * np.where(d % 2 == 0, -1.0, 1.0)[None, :]
    ropec = np.ascontiguousarray(np.concatenate([C, C], axis=1).T.astype(np.float32))
    ropes = np.ascontiguousarray(np.concatenate([S, S], axis=1).T.astype(np.float32))
    ki = np.arange(128)[:, None]
    qi = np.arange(128)[None, :]
    NEG = -30000.0
    mL = np.where(qi <= ki, 0.0, NEG)
    mR = np.where(ki <= qi, 0.0, NEG)
    mL0 = mL if half == 1 else np.full_like(mL, NEG)
    mR31 = mR if half == 0 else np.full_like(mR, NEG)
    masks = np.stack([np.tile(m, (1, 4)) for m in (mL, mR, mL0, mR31)], axis=1).astype(np.float32)
    hmask = np.tile(np.array([[1.0 if half == 1 else 0.0, 1.0 if half == 0 else 0.0]], np.float32), (128, 1))
    return dict(mre=mre, mim=mim, ropec=ropec, ropes=ropes, masks=masks, hmask=hmask)


def _host_inputs(inp):
    C = _consts()
    g = {k: np.asarray(v) for k, v in inp.items()}
    w_in = g['w_in'][0]
    hy = np.arange(0, 1536)
    x0c, x1c, vc = hy[0:512], hy[512:1024], hy[1024:1536]
    qc = 1536 + np.arange(512)
    kc = 2048 + np.arange(128)
    vac = 2176 + np.arange(128)
    gc = 2304 + np.arange(2048)
    qperm = np.concatenate([np.concatenate([qc[j * 64:(j + 1) * 64], qc[(4 + j) * 64:(5 + j) * 64]]) for j in range(4)])
    sw = np.arange(64) ^ 1
    qsw = np.concatenate([np.concatenate([qc[j * 64:(j + 1) * 64][sw], qc[(4 + j) * 64:(5 + j) * 64][sw]]) for j in range(4)])
    ksw = np.concatenate([kc[0:64][sw], kc[64:128][sw]])
    cols = np.concatenate([x1c, vc, x0c, qperm, qsw, kc, ksw, vac, gc])
    assert cols.shape[0] == NCOL
    w_in_p = np.ascontiguousarray(w_in[:, cols])
    conv_w = g['hy_conv_w'][0][:, 0, :]
    conv_b = g['hy_conv_b'][0]
    hcols = np.concatenate([x1c, vc, x0c])
    cw = np.concatenate([conv_w[:, hcols], conv_b[None, hcols]], axis=0)
    cw = np.ascontiguousarray(cw.reshape(4, 12, 128).transpose(2, 1, 0))
    w1s = np.zeros((66, 128), np.float32)
    w1s[0:33, 0:64] = g['hy_w1'][0]
    w1s[33:66, 64:128] = g['hy_w1'][0]
    w2s = np.zeros((128, 128), np.float32)
    w2s[0:64, 0:64] = g['hy_w2'][0]
    w2s[64:128, 64:128] = g['hy_w2'][0]
    w3s = np.zeros((128, 128), np.float32)
    w3s[0:64, 0:64] = g['hy_w3'][0]
    w3s[64:128, 64:128] = g['hy_w3'][0]
    w4 = g['hy_w4'][0]
    w4s = np.ascontiguousarray(np.concatenate([w4[:, :512], w4[:, 512:]], axis=0))
    hyv = np.stack([np.tile(g[n][0], 2) for n in ('hy_b1', 'hy_b2', 'hy_b3', 'hy_freq')], axis=1).astype(np.float32)
    hbias = np.ascontiguousarray(np.tile(g['hy_bias'][0][None, :], (128, 1)))
    sinks = g['attn_sinks'][0]
    sinks_b = np.ascontiguousarray(np.tile(sinks[None, :], (128, 1)))
    shared = dict(
        ada_w=g['ada_w'][0], ada_b=np.ascontiguousarray(np.tile(g['ada_b'][0][None, :], (2, 1))),
        w_in_p=w_in_p, cw=cw, w1s=w1s, w2s=w2s, w3s=w3s, w4s=w4s, hyv=hyv, hbias=hbias, sinks=sinks_b,
        w_bh=g['w_branch_hy'][0], w_ba=g['w_branch_attn'][0], w_o=g['w_out'][0],
        ln1=np.ascontiguousarray(np.stack([g['ln1_g'][0], g['ln1_b'][0]])),
        ln2=np.ascontiguousarray(np.stack([g['ln2_g'][0], g['ln2_b'][0]])),
        w_gr=np.ascontiguousarray(np.concatenate([g['w_group'][0], g['w_router'][0]], axis=1)),
        b_gr=np.ascontiguousarray(np.tile(np.concatenate([g['b_group'][0], g['b_router'][0]])[None, :], (128, 1))),
        w_ge=g['w_gate_e'][0], w_ue=g['w_up_e'][0], w_de=g['w_down_e'][0],
        fc=C['fc'], gk=C['gk'], cc1=C['cc1'], cc2=C['cc2'], zz=C['zz'], dec=C['dec'], ident=C['ident'], sel=C['sel'],
    )
    maps = []
    x = g['x']
    for core in range(8):
        b, half = core // 2, core % 2
        t0 = half * LO
        xe = np.zeros((NEXT, D), np.float32)
        lo, hi = max(0, t0 - 128), min(L, t0 + LO + 128)
        xe[lo - (t0 - 128):hi - (t0 - 128)] = x[b, lo:hi]
        cc = np.stack([g['c'][b], g['c_ctx']], axis=1).reshape(8, 128, 2).transpose(1, 0, 2)
        m = dict(shared)
        m.update(xfull=np.ascontiguousarray(x[b]), xext=xe, cc=np.ascontiguousarray(cc.astype(np.float32)),
                 ctx=np.ascontiguousarray(g['ctx'][b]))
        m.update(_core_consts(half))
        maps.append(m)
    return maps


IN_SPECS = dict(
    xfull=([L, D], F32), xext=([NEXT, D], F32), cc=([128, 8, 2], F32), ctx=([256, D], F32),
    ada_w=([D, 6 * D], F32), ada_b=([2, 6 * D], F32), w_in_p=([D, NCOL], F32), cw=([128, 12, 4], F32),
    w1s=([66, 128], F32), w2s=([128, 128], F32), w3s=([128, 128], F32), w4s=([128, 512], F32),
    hyv=([128, 4], F32), hbias=([128, 512], F32), sinks=([128, 8], F32),
    w_bh=([512, D], F32), w_ba=([512, D], F32), w_o=([D, D], F32), ln1=([2, D], F32), ln2=([2, D], F32),
    w_gr=([D, 20], F32), b_gr=([128, 20], F32), w_ge=([NE, D, DE], F32), w_ue=([NE, D, DE], F32), w_de=([NE, DE, D], F32),
    fc=([128, 3 * NKF], F32), gk=([128, NKF, 2, 128], BF16), cc1=([128, 256], F32), cc2=([128, 256], F32),
    zz=([66, L], F32), dec=([NG, 128, 128, GCH], F32), ident=([128, 128], F32),
    mre=([NKF, 128, 32], F32), mim=([NKF, 128, 32], F32), ropec=([128, NEXT], F32), ropes=([128, NEXT], F32),
    masks=([128, 4, 512], F32), sel=([16, 16, 128], F32), hmask=([128, 2], F32),
)
SCRATCH = dict(
    modv=([2, 6 * D], F32), vx=([DH, 17 * 512], BF16), yc=([DH, LO], BF16), gt=([2 * D, LO], BF16),
    x1=([LO, D], F32), kvc=([256, 256], F32), wgu=([NE, 2, 128, 8 * DE], BF16), wdb=([NE, 128, 2 * D], BF16),
    dbg_yat=([LO, 512], BF16), dbg_mix=([LO, D], F32), dbg_yh=([512, LO], BF16),
    dbg_qr=([128, 4 * LO], BF16), dbg_kr=([128, NEXT], BF16), dbg_vt=([128, 34 * 130], BF16), dbg_kc=([128, 256], BF16), dbg_vc=([128, 260], BF16),
)


def ln_tile(k, xt, xn, st, mv, rstd, kx, sl, kout=None):
    for h in range(2):
        k.op('dve', lambda e: e.bn_stats(out=st[:, h, :], in_=xt[:, h * 512:(h + 1) * 512]), reads=[kx], writes=[('st', sl)])
    k.op('dve', lambda e: e.bn_aggr(out=mv[:], in_=st[:].rearrange("p a b -> p (a b)")), reads=[('st', sl)], writes=[('mv', sl)])
    k.op('act', lambda e: e.activation(out=rstd[:], in_=mv[:, 1:2], func=AF.Sqrt, bias=EPS, scale=1.0),
         reads=[('mv', sl)], writes=[('rstd', sl)])
    k.op('dve', lambda e: e.reciprocal(out=rstd[:], in_=rstd[:]), reads=[('rstd', sl)], writes=[('rstd', sl)])
    k.op('dve', lambda e: e.tensor_scalar(out=xn[:], in0=xt[:], scalar1=mv[:, 0:1], scalar2=rstd[:, 0:1],
                                          op0=ALU.subtract, op1=ALU.mult),
         reads=[kx, ('mv', sl), ('rstd', sl)], writes=[kout if kout is not None else ('xn', sl)])


def load_pvec(k, dst, src_row, key, q='sp'):
    k.dma(q, key, dst, src_row.rearrange("(kc p) -> p kc", p=128), writes=[key], slow=True)


def phase1(k, T):
    k.begin()
    cs = k.sbuf('cs', [128, 8, 2], F32)
    csl = k.sbuf('csl', [128, 8, 2], F32)
    ab = k.sbuf('ab', [2, 6 * D], F32)
    mo = k.sbuf('mo', [2, 6 * D], F32)
    wb = [k.sbuf('aw%d' % i, [128, 8, 512], F32) for i in range(2)]
    ps = [k.psum('p1_%d' % i, [128, 512], F32) for i in range(2)]
    k.dma('sp', 'cs', cs[:], T['cc'], writes=['cs'])
    k.dma('sp', 'ab', ab[:], T['ada_b'], writes=['ab'])
    k.op('act', lambda e: e.activation(out=csl[:], in_=cs[:], func=AF.Silu), reads=['cs'], writes=['csl'])
    for n in range(12):
        s = n % 2
        k.dma('sp', ('aw', s), wb[s][:],
              T['ada_w'][:, n * 512:(n + 1) * 512].rearrange("(kc p) n -> p kc n", p=128), writes=[('aw', s)])
        for kc in range(8):
            k.op('pe', lambda e: e.matmul(ps[s][0:2, :], lhsT=csl[:, kc, :], rhs=wb[s][:, kc, :], start=(kc == 0), stop=(kc == 7)),
                 reads=['csl', ('aw', s)], writes=[('p1', s)])
        k.op('dve', lambda e: e.tensor_tensor(out=mo[:, n * 512:(n + 1) * 512], in0=ps[s][0:2, :], in1=ab[:, n * 512:(n + 1) * 512], op=ALU.add),
             reads=[('p1', s), 'ab'], writes=['mo'])
    k.dma('sp', 'modv', T['modv'], mo[:], reads=['mo'], writes=['modv_d'])
    k.end()


def phase2(k, T):
    k.begin()
    identf = k.sbuf('identf', [128, 128], F32)
    ident = k.sbuf('ident', [128, 128], BF16)
    s1p = k.sbuf('s1p', [128, 8], F32)
    sh1p = k.sbuf('sh1p', [128, 8], F32)
    wst = [k.sbuf('wst%d' % i, [128, 8, 512], F32) for i in range(2)]
    wA = k.sbuf('wA', [128, 8, 1024], BF16)
    cw = k.sbuf('cw', [128, 12, 4], F32)
    xt = [k.sbuf('xt%d' % i, [128, D], F32) for i in range(2)]
    xn = [k.sbuf('xn%d' % i, [128, D], BF16) for i in range(2)]
    st = [k.sbuf('st%d' % i, [128, 2, 6], F32) for i in range(2)]
    mv = [k.sbuf('mv%d' % i, [128, 2], F32) for i in range(2)]
    rstd = [k.sbuf('rstd%d' % i, [128, 1], F32) for i in range(2)]
    hT = [k.sbuf('hT%d' % i, [128, 8, 512], BF16) for i in range(2)]
    ub = [k.sbuf('ub%d' % i, [128, 8, 514], F32) for i in range(2)]
    tc = k.sbuf('tc', [128, 8, 512], F32)
    vxs = [k.sbuf('vxs%d' % i, [128, 4, 512], BF16) for i in range(2)]
    pT = [k.psum('pT%d' % i, [128, 8, 128], BF16) for i in range(2)]
    pu = [k.psum('pu%d' % i, [128, 512], F32) for i in range(2)]

    cstb = [k.sbuf('cst%d' % i, [128, 2048], F32) for i in range(4)]
    cbf = [k.sbuf('cbf%d' % i, [128, 2048], BF16) for i in range(6)]
    stg_bufs = [(cstb[i][:], ('cst', i)) for i in range(4)] + [(wst[j][:].rearrange("p a b -> p (a b)")[:, 0:2048], ('wst', j)) for j in range(2)]
    jobs = []
    for e_ in range(NE):
        jobs.append((T['w_ge'][e_].rearrange("(kc p) f -> p kc f", p=128), 8, T['wgu'][e_, 0]))
        jobs.append((T['w_ue'][e_].rearrange("(kc p) f -> p kc f", p=128), 8, T['wgu'][e_, 1]))
        jobs.append((T['w_de'][e_].rearrange("(fc p) d -> p fc d", p=128), 2, T['wdb'][e_]))
    jl = [0]
    jc = [0]
    outq = []

    def conv_load():
        if jl[0] >= len(jobs):
            return
        src, a_, dst = jobs[jl[0]]
        buf, key = stg_bufs[jl[0] % 6]
        jl[0] += 1
        k.dma('sp', key, buf.rearrange("p (a b) -> p a b", a=a_), src, writes=[key])

    def conv_cast():
        while len(outq) > 2:
            i_, dst_ = outq.pop(0)
            k.dma('sp', ('cbo', i_), dst_, cbf[i_][:], reads=[('cbf', i_)], writes=['wconv_d'])
        if jc[0] >= jl[0]:
            return
        src, a_, dst = jobs[jc[0]]
        i = jc[0] % 6
        buf, key = stg_bufs[i]
        jc[0] += 1
        k.op('act', lambda en: en.activation(out=cbf[i][:], in_=buf, func=AF.Copy), reads=[key], writes=[('cbf', i)])
        outq.append((i, dst))

    def conv_flush():
        while jc[0] < len(jobs):
            if jl[0] < len(jobs):
                conv_load()
            conv_cast()
        while outq:
            i_, dst_ = outq.pop(0)
            k.dma('sp', ('cbo', i_), dst_, cbf[i_][:], reads=[('cbf', i_)], writes=['wconv_d'])

    k.dma('sp', 'identf', identf[:], T['ident'], writes=['identf'])
    k.op('act', lambda e: e.activation(out=ident[:], in_=identf[:], func=AF.Copy), reads=['identf'], writes=['ident'])
    load_pvec(k, sh1p[:], T['modv'][0, 0:D], 'sh1p')
    load_pvec(k, s1p[:], T['modv'][0, D:2 * D], 's1p')
    k.op('dve', lambda e: e.tensor_scalar(out=s1p[:], in0=s1p[:], scalar1=1.0, scalar2=None, op0=ALU.add), reads=['s1p'], writes=['s1p'])
    k.dma('sp', 'cw', cw[:], T['cw'], writes=['cw'])
    shb = k.sbuf('shb', [128, 8], BF16)
    wAu = k.sbuf('wAu', [128, 8, 1024], BF16)
    c0 = k.sbuf('c0', [128, 8], F32)
    k.op('dve', lambda e: e.tensor_copy(out=shb[:], in_=sh1p[:]), reads=['sh1p'], writes=['shb'])
    for j in range(2):
        k.dma('sp', ('wst', j), wst[j][:], T['w_in_p'][:, C_X1V + j * 512:C_X1V + (j + 1) * 512].rearrange("(kc p) n -> p kc n", p=128),
              writes=[('wst', j)])
        k.op('dve', lambda e: e.tensor_copy(out=wAu[:, :, j * 512:(j + 1) * 512], in_=wst[j][:]), reads=[('wst', j)], writes=['wAu'])
        for kc in range(8):
            k.op('act', lambda e: e.activation(out=wA[:, kc, j * 512:(j + 1) * 512], in_=wst[j][:, kc, :], func=AF.Copy, scale=s1p[:, kc:kc + 1]),
                 reads=[('wst', j), 's1p'], writes=['wA'])
    for cc in range(8):
        for kc in range(8):
            k.op('pe', lambda e: e.matmul(pu[0][:, cc:cc + 1], lhsT=wAu[:, kc, cc * 128:(cc + 1) * 128], rhs=shb[:, kc:kc + 1], start=(kc == 0), stop=(kc == 7)),
                 reads=['wAu', 'shb'], writes=[('pu', 0)])
    k.op('dve', lambda e: e.tensor_copy(out=c0[:], in_=pu[0][:, 0:8]), reads=[('pu', 0)], writes=['c0'])
    k.op('pool', lambda e: e.memset(ub[0][:, :, 0:2], 0.0), writes=[('ub', 0, c_) for c_ in range(8)])

    xv = T['vx'].rearrange("(cc p) t -> p cc t", p=128)
    ti = [0]

    pu = pu + [k.psum('pu%d' % i, [128, 512], F32) for i in range(2, 4)]

    def prep_ln(i, j):
        sl = ti[0] % 2
        ti[0] += 1
        r0 = i * 512 + j * 128
        k.dma('sp', ('xt', sl), xt[sl][:], T['xfull'][r0:r0 + 128, :], writes=[('xt', sl)])
        ln_tile(k, xt[sl], xn[sl], st[sl], mv[sl], rstd[sl], ('xt', sl), sl)
        return sl

    def prep_tr(sl):
        for kc in range(8):
            k.op('pe', lambda e: e.transpose(out=pT[sl][:, kc, :], in_=xn[sl][:, kc * 128:(kc + 1) * 128], identity=ident[:]),
                 reads=[('xn', sl), 'ident'], writes=[('pT', sl)])

    def prep_mod(i, j, sl):
        s = i % 2
        k.op('act', lambda e: e.activation(out=hT[s][:, :, j * 128:(j + 1) * 128], in_=pT[sl][:, :, :], func=AF.Copy),
             reads=[('pT', sl)], writes=[('hT', s)])

    def conv_cc(s, cc):
        k.op('act', lambda e: e.activation(out=tc[:, cc, :], in_=ub[s][:, cc, 1:513], func=AF.Identity,
                                           scale=cw[:, cc, 1:2], bias=cw[:, cc, 3:4]),
             reads=[('ub', s, cc), 'cw'], writes=[('tc', cc)])
        k.op('dve', lambda e: e.scalar_tensor_tensor(out=tc[:, cc, :], in0=ub[s][:, cc, 0:512], scalar=cw[:, cc, 0:1], in1=tc[:, cc, :],
                                                     op0=ALU.mult, op1=ALU.add),
             reads=[('ub', s, cc), 'cw', ('tc', cc)], writes=[('tc', cc)])
        k.op('dve', lambda e: e.scalar_tensor_tensor(out=tc[:, cc, :], in0=ub[s][:, cc, 2:514], scalar=cw[:, cc, 2:3], in1=tc[:, cc, :],
                                                     op0=ALU.mult, op1=ALU.add),
             reads=[('ub', s, cc), 'cw', ('tc', cc)], writes=[('tc', cc)])

    for _ in range(3):
        conv_load()
    for j in range(4):
        sl0 = prep_ln(0, j)
        prep_tr(sl0)
        prep_mod(0, j, sl0)
    for i in range(17):
        s = i % 2
        if i < 16:
            for pr in range(4):
                prep = i + 1 < 16
                if prep:
                    slp = prep_ln(i + 1, pr)
                for cc in (2 * pr, 2 * pr + 1):
                    ps_ = cc % 4
                    for kc in range(8):
                        k.op('pe', lambda e: e.matmul(pu[ps_][:], lhsT=wA[:, kc, cc * 128:(cc + 1) * 128], rhs=hT[s][:, kc, :],
                                                      start=(kc == 0), stop=(kc == 7)),
                             reads=['wA', ('hT', s)], writes=[('pu', ps_)])
                if prep:
                    prep_tr(slp)
                for cc in (2 * pr, 2 * pr + 1):
                    ps_ = cc % 4
                    k.op('act', lambda e: e.activation(out=ub[s][:, cc, 2:514], in_=pu[ps_][:], func=AF.Identity, bias=c0[:, cc:cc + 1], scale=1.0),
                         reads=[('pu', ps_), 'c0'], writes=[('ub', s, cc)])
                    conv_cc(s, cc)
                if prep:
                    prep_mod(i + 1, pr, slp)
                if pr < 3:
                    conv_cast()
                    conv_load()
        else:
            k.op('pool', lambda e: e.memset(ub[s][:, :, 2:514], 0.0), writes=[('ub', s, c_) for c_ in range(8)])
            for cc in range(8):
                conv_cc(s, cc)
        k.op('dve', lambda e: e.tensor_tensor(out=vxs[s][:], in0=tc[:, 0:4, :], in1=tc[:, 4:8, :], op=ALU.mult),
             reads=[('tc', c_) for c_ in range(8)], writes=[('vxs', s)])
        k.dma('sp', ('vxo', s), xv[:, :, i * 512:(i + 1) * 512], vxs[s][:], reads=[('vxs', s)], writes=['vx_d'])
        if i < 16:
            k.op('pool', lambda e: e.tensor_copy(out=ub[1 - s][:, :, 0:2], in_=ub[s][:, :, 512:514]), reads=[('ub', s, c_) for c_ in range(8)], writes=[('ub', 1 - s, c_) for c_ in range(8)])
    conv_flush()
    k.end()


PHASES = {}
DEBUG = False


def build_nc(test=None):
    nc = bass.Bass("TRN2", target_bir_lowering=False)
    T = {}
    ext_in = set(test[1]) if test else set()
    ext_out = set(test[2]) if test else set()
    for n, (shape, dt) in IN_SPECS.items():
        T[n] = nc.dram_tensor(n, shape, dt, kind="ExternalInput").ap()
    for n, (shape, dt) in SCRATCH.items():
        kind = "ExternalInput" if n in ext_in else ("ExternalOutput" if n in ext_out else "Internal")
        T[n] = nc.dram_tensor(n, shape, dt, kind=kind).ap()
    T['out'] = nc.dram_tensor("out", [LO, D], F32, kind="ExternalOutput").ap()
    with ExitStack() as es:
        k = K(nc, es)
        names = test[0] if test else ['phase1', 'phase2', 'phase3', 'phase4', 'phase5']
        for n in names:
            PHASES[n](k, T)
        k.barrier()
    return nc


PHASES.update(phase1=phase1, phase2=phase2)


def phase3(k, T):
    k.begin()
    HD = k.sbuf('HD', [128, 128, 128], BF16)
    gk = k.sbuf('gk', [128, NKF, 2, 128], BF16)
    fc = k.sbuf('fc', [128, 3 * NKF], BF16)
    cc1 = k.sbuf('cc1', [128, 256], BF16)
    cc2 = k.sbuf('cc2', [128, 256], BF16)
    mre = k.sbuf('mre', [128, 128, 32], BF16)
    mim = k.sbuf('mim', [128, 128, 32], BF16)
    w4s = k.sbuf('w4s', [128, 512], BF16)
    hbias = k.sbuf('hbias', [128, 512], F32)
    ones = k.sbuf('ones', [128, 128], F32)
    banks = [k.psum('bk%d' % i, [128, 512], F32) for i in range(8)]

    def bank(i):
        return banks[i], ('bk', i)

    k.dma('sp', 'gk', gk[:], T['gk'], writes=['gk'])
    k.dma('sp', 'hbias', hbias[:], T['hbias'], writes=['hbias'])
    k.op('pool', lambda e: e.memset(ones[:], 1.0), writes=['ones'])
    k.op('pool', lambda e: e.memset(HD[:], 0.0), writes=['HD'])

    outer = k.pes
    k.pes = ExitStack()
    k.pes.__enter__()
    stg = k.sbuf('stg', [128, 4096], F32)
    w1s = k.sbuf('w1s', [66, 128], F32)
    w2s = k.sbuf('w2s', [128, 128], F32)
    w3s = k.sbuf('w3s', [128, 128], F32)
    hyv = k.sbuf('hyv', [128, 4], F32)
    fr2 = k.sbuf('fr2', [128, 1], F32)
    frb = k.sbuf('frb', [128, 3], F32)
    zc = [k.sbuf('zc%d' % i, [66, 512], F32) for i in range(2)]
    rrs = [k.sbuf('rr%d' % i, [128, 512], F32) for i in range(2)]
    r2s = [k.sbuf('r2%d' % i, [128, 512], F32) for i in range(2)]
    hhs = [[k.sbuf('hh%d_%d' % (i, j), [128, 512], F32) for j in range(2)] for i in range(2)]

    def load_cast(dst, src, n, key):
        k.dma('sp', 'stg', stg[:src.shape[0], 0:n], src, writes=['stg'])
        k.op('act', lambda e: e.activation(out=dst, in_=stg[:src.shape[0], 0:n], func=AF.Copy), reads=['stg'], writes=[key])

    load_cast(fc[:], T['fc'], 3 * NKF, 'fc')
    load_cast(cc1[:], T['cc1'], 256, 'cc1')
    load_cast(cc2[:], T['cc2'], 256, 'cc2')
    load_cast(w4s[:], T['w4s'], 512, 'w4s')
    load_cast(mre[0:NKF].rearrange("p a b -> p (a b)"), T['mre'].rearrange("p a b -> p (a b)"), 4096, 'mre')
    load_cast(mim[0:NKF].rearrange("p a b -> p (a b)"), T['mim'].rearrange("p a b -> p (a b)"), 4096, 'mim')
    k.dma('sp', 'w1s', w1s[:], T['w1s'], writes=['w1s'])
    k.dma('sp', 'w2s', w2s[:], T['w2s'], writes=['w2s'])
    k.dma('sp', 'w3s', w3s[:], T['w3s'], writes=['w3s'])
    k.dma('sp', 'hyv', hyv[:], T['hyv'], writes=['hyv'])
    k.op('dve', lambda e: e.tensor_scalar(out=fr2[:], in0=hyv[:, 3:4], scalar1=1.0 / TWO_PI, scalar2=None, op0=ALU.mult),
         reads=['hyv'], writes=['fr2'])
    k.op('dve', lambda e: e.tensor_scalar(out=frb[:], in0=hyv[:, 0:3], scalar1=fr2[:, 0:1], scalar2=None, op0=ALU.mult),
         reads=['hyv', 'fr2'], writes=['frb'])

    def sin_layer(li, pin, kin, out_fn, s):
        rr, r2 = rrs[s], r2s[s]
        k.op('dve', lambda e: e.tensor_scalar(out=rr[:], in0=pin, scalar1=fr2[:, 0:1], scalar2=frb[:, li:li + 1], op0=ALU.mult, op1=ALU.add),
             reads=[kin, 'fr2', 'frb'], writes=[('rr', s)])
        k.op('dve', lambda e: e.scalar_tensor_tensor(out=r2[:], in0=rr[:], scalar=-0.5, in1=rr[:], op0=ALU.is_lt, op1=ALU.add),
             reads=[('rr', s)], writes=[('r2', s)])
        k.op('dve', lambda e: e.scalar_tensor_tensor(out=r2[:], in0=rr[:], scalar=0.5, in1=r2[:], op0=ALU.is_gt, op1=ALU.subtract),
             reads=[('rr', s), ('r2', s)], writes=[('r2', s)])
        out_fn()

    def mlp_layer(ch, li):
        s = ch % 2
        r2 = r2s[s]
        hh = hhs[s]
        bk_, kb_ = bank(2 * li + s)
        if li == 0:
            k.dma('sp', ('zc', s), zc[s][:], T['zz'][:, ch * 512:(ch + 1) * 512], writes=[('zc', s)])
            k.op('pe', lambda e: e.matmul(bk_[:], lhsT=w1s[:], rhs=zc[s][:], start=True, stop=True), reads=['w1s', ('zc', s)], writes=[kb_])
        elif li == 1:
            k.op('pe', lambda e: e.matmul(bk_[:], lhsT=w2s[:], rhs=hh[0][:], start=True, stop=True), reads=['w2s', ('hh', s, 0)], writes=[kb_])
        else:
            k.op('pe', lambda e: e.matmul(bk_[:], lhsT=w3s[:], rhs=hh[1][:], start=True, stop=True), reads=['w3s', ('hh', s, 1)], writes=[kb_])

        def out_fn():
            if li < 2:
                k.op('act', lambda e: e.activation(out=hh[li][:], in_=r2[:], func=AF.Sin, scale=-TWO_PI), reads=[('r2', s)], writes=[('hh', s, li)])
            else:
                for hf in range(2):
                    m0 = 4 * ch + 64 * hf
                    k.op('act', lambda e: e.activation(out=HD[64 * hf:64 * hf + 64, :, m0:m0 + 4].rearrange("p n m -> p m n"),
                                                       in_=r2[64 * hf:64 * hf + 64, :].rearrange("p (m n) -> p m n", m=4),
                                                       func=AF.Sin, scale=-TWO_PI), reads=[('r2', s)], writes=['HD'])
        sin_layer(li, bk_[:], kb_, out_fn, s)

    for ch in range(0, 16, 2):
        for li in range(3):
            mlp_layer(ch, li)
            mlp_layer(ch + 1, li)
    k.barrier()
    k.pes.__exit__(None, None, None)
    k.pes = outer

    dec = [k.sbuf('dec%d' % i, [128, 32, GCH], F32) for i in range(2)]
    XH = k.sbuf('XH', [128, GCH, 128], BF16)
    U = k.sbuf('U', [128, 16384], BF16)
    A_sb = U[:, 0:3 * NKF * GCH].rearrange("p (c a k) -> p c a k", c=GCH, a=3)
    D_sb = U[:, 0:2 * 128 * GCH].rearrange("p (c a n) -> p c a n", c=GCH, a=2)
    SQ = U[:, 0:GCH * 128].rearrange("p (c n) -> p c n", c=GCH)
    Hf = k.sbuf('Hf', [128, 2, NKF, GCH], BF16)
    Y = k.sbuf('Y', [128, 2, NKF, GCH], BF16)
    yout = k.sbuf('yout', [GCH, LO], BF16)
    ssum = k.sbuf('ssum', [128, GCH], F32)
    rn = k.sbuf('rn', [128, GCH], F32)
    rnb = k.sbuf('rnb', [128, 8, GCH], F32)
    bb = k.sbuf('bb', [128, 8, GCH], F32)
    tt = [k.sbuf('tt%d' % i, [128, 8, GCH], BF16) for i in range(4)]
    xsb = [k.sbuf('xsb%d' % i, [128, 2, 8, GCH], BF16) for i in range(2)]
    tf = k.sbuf('tf', [128, 8, GCH], F32)
    vxv = T['vx']

    SQY = Y[:].rearrange("p a k c -> p (a k c)")[:, 0:GCH * 128].rearrange("p (c n) -> p c n", c=GCH)

    def build_batch(g, nb):
        c0 = g * GCH
        dq = nb // 4
        ds = (g * 4 + dq) % 2
        if nb % 4 == 0:
            k.dma('sp', ('dec', ds), dec[ds][:], T['dec'][g, :, dq * 32:(dq + 1) * 32, :], writes=[('dec', ds)])
        bk, kbk = bank(nb % 2)
        pf = bk[:, 0:8 * GCH].rearrange("p (j c) -> p j c", j=8)
        for j in range(8):
            n2 = nb * 8 + j
            k.op('pe', lambda e: e.matmul(pf[:, j, :], lhsT=HD[:, n2, :], rhs=w4s[:, c0:c0 + GCH], start=True, stop=True),
                 reads=['HD', 'w4s'], writes=[kbk])
        k.op('dve', lambda e: e.tensor_tensor(out=XH[:, :, nb * 8:(nb + 1) * 8].rearrange("p c n -> p n c"), in0=pf,
                                              in1=dec[ds][:, (nb % 4) * 8:(nb % 4) * 8 + 8, :], op=ALU.mult),
             reads=[kbk, ('dec', ds)], writes=['XH'])

    def build_stats(g):
        k.op('act', lambda e: e.activation(out=SQY, in_=XH[:], func=AF.Square), reads=['XH'], writes=['Y'])
        k.op('dve', lambda e: e.tensor_reduce(out=ssum[:], in_=SQY, axis=AX.X, op=ALU.add), reads=['Y'], writes=['ssum'])

    for nb in range(16):
        build_batch(0, nb)
    build_stats(0)
    for g in range(NG):
        c0 = g * GCH
        def stage1(nrows):
            for cp in range(GCH // 2):
                bk, kbk = bank(2 + cp % 2)
                pa = bk[:, :].rearrange("p (j x) -> p j x", j=2)
                for j in range(2):
                    c = cp * 2 + j
                    k.op('pe', lambda e: e.matmul(pa[:, j, 0:3 * NKF], lhsT=XH[0:nrows, c, :], rhs=fc[0:nrows, :], start=True, stop=True),
                         reads=['XH', 'fc'], writes=[kbk])
                k.op('act', lambda e: e.activation(out=A_sb[:, cp * 2:cp * 2 + 2, :, :].rearrange("p c a k -> p c (a k)"),
                                                   in_=pa[:, :, 0:3 * NKF], func=AF.Copy), reads=[kbk], writes=['U'])

        def stage2(consume):
            nbat = (NKF + 3) // 4
            for kb in range(nbat):
                nk = min(4, NKF - kb * 4)
                bx, kbx = bank(4 + kb % 4)
                xv = bx[:, :].rearrange("p (j c a) -> p j c a", j=4, a=2)
                for j in range(nk):
                    k1 = kb * 4 + j
                    k.op('pe', lambda e: e.matmul(xv[:, j, :, :], lhsT=gk[:, k1, 0, :], rhs=A_sb[:, :, 1:3, k1], start=True, stop=False),
                         reads=['gk', 'U'], writes=[kbx])
                    k.op('pe', lambda e: e.matmul(xv[:, j, :, :], lhsT=gk[:, k1, 1, :], rhs=A_sb[:, :, 0:2, k1], start=False, stop=True),
                         reads=['gk', 'U'], writes=[kbx])
                consume(kb, nk, xv[:, :, :, 0], kbx, xv[:, :, :, 1], kbx)

        stage1(128)
        bk, kbk = bank(1)
        k.op('pe', lambda e: e.matmul(bk[:, 0:GCH], lhsT=ones[:], rhs=ssum[:], start=True, stop=True), reads=['ones', 'ssum'], writes=[kbk])
        k.op('act', lambda e: e.activation(out=rn[:], in_=bk[:, 0:GCH], func=AF.Sqrt, bias=1e-6, scale=1.0), reads=[kbk], writes=['rn'])
        k.op('dve', lambda e: e.reciprocal(out=rn[:], in_=rn[:]), reads=['rn'], writes=['rn'])
        for j in range(8):
            k.op('dve', lambda e: e.tensor_copy(out=rnb[:, j, :], in_=rn[:]), reads=['rn'], writes=['rnb'])
            k.op('pool', lambda e: e.tensor_copy(out=bb[:, j, :], in_=hbias[:, c0:c0 + GCH]), reads=['hbias'], writes=['bb'])


        def filt_consume(kb, nk, xr, kbr, xi, kbi):
            ks = slice(kb * 4, kb * 4 + nk)
            k.op('dve', lambda e: e.tensor_tensor(out=tf[:, 0:nk, :], in0=xr[:, 0:nk, :], in1=rnb[:, 0:nk, :], op=ALU.mult),
                 reads=[kbr, 'rnb'], writes=['tf'])
            k.op('pool', lambda e: e.tensor_tensor(out=Hf[:, 0, ks, :], in0=tf[:, 0:nk, :], in1=bb[:, 0:nk, :], op=ALU.add),
                 reads=['tf', 'bb'], writes=['Hf'])
            k.op('dve', lambda e: e.tensor_tensor(out=Hf[:, 1, ks, :], in0=xi[:, 0:nk, :], in1=rnb[:, 0:nk, :], op=ALU.mult),
                 reads=[kbi, 'rnb'], writes=['Hf'])
        stage2(filt_consume)

        k.dma('sp', 'xh', XH[0:64, :, :], vxv[c0:c0 + GCH, 1:1 + L].rearrange("c (a n) -> a c n", n=128), reads=[], writes=['XH'])
        stage1(64)

        def data_consume(kb, nk, xr, kbr, xi, kbi):
            ks = slice(kb * 4, kb * 4 + nk)
            hr = Hf[:, 0, ks, :]
            hi = Hf[:, 1, ks, :]
            q = kb % 2
            k.op('act', lambda e: e.activation(out=xsb[q][:, 0, 0:nk, :], in_=xr[:, 0:nk, :], func=AF.Copy), reads=[kbr], writes=[('xsb', q, 0)])
            k.op('act', lambda e: e.activation(out=xsb[q][:, 1, 0:nk, :], in_=xi[:, 0:nk, :], func=AF.Copy), reads=[kbi], writes=[('xsb', q, 1)])
            xrb = xsb[q][:, 0, 0:nk, :]
            xib = xsb[q][:, 1, 0:nk, :]
            k.op('dve', lambda e: e.tensor_tensor(out=tt[0][:, 0:nk, :], in0=xrb, in1=hr, op=ALU.mult), reads=[('xsb', q, 0), 'Hf'], writes=[('tt', 0)])
            k.op('dve', lambda e: e.tensor_tensor(out=tt[1][:, 0:nk, :], in0=xib, in1=hi, op=ALU.mult), reads=[('xsb', q, 1), 'Hf'], writes=[('tt', 1)])
            k.op('dve', lambda e: e.tensor_tensor(out=tt[2][:, 0:nk, :], in0=xrb, in1=hi, op=ALU.mult), reads=[('xsb', q, 0), 'Hf'], writes=[('tt', 2)])
            k.op('dve', lambda e: e.tensor_tensor(out=tt[3][:, 0:nk, :], in0=xib, in1=hr, op=ALU.mult), reads=[('xsb', q, 1), 'Hf'], writes=[('tt', 3)])
            k.op('dve', lambda e: e.tensor_tensor(out=Y[:, 0, ks, :], in0=tt[0][:, 0:nk, :], in1=tt[1][:, 0:nk, :],
                                                  op=ALU.subtract), reads=[('tt', 0), ('tt', 1)], writes=['Y'])
            k.op('dve', lambda e: e.tensor_tensor(out=Y[:, 1, ks, :], in0=tt[2][:, 0:nk, :], in1=tt[3][:, 0:nk, :],
                                                  op=ALU.add), reads=[('tt', 2), ('tt', 3)], writes=['Y'])
        stage2(data_consume)

        for cp in range(GCH // 2):
            bk, kbk = bank(cp % 2)
            pd = bk[:, :].rearrange("p (j x) -> p j x", j=2)
            for j in range(2):
                c = cp * 2 + j
                k.op('pe', lambda e: e.matmul(pd[0:NKF, j, :], lhsT=Y[:, 0, :, c], rhs=cc1[:], start=True, stop=False),
                     reads=['Y', 'cc1'], writes=[kbk])
                k.op('pe', lambda e: e.matmul(pd[0:NKF, j, :], lhsT=Y[:, 1, :, c], rhs=cc2[:], start=False, stop=True),
                     reads=['Y', 'cc2'], writes=[kbk])
            k.op('act', lambda e: e.activation(out=D_sb[0:NKF, cp * 2:cp * 2 + 2, :, :].rearrange("p c a n -> p c (a n)"),
                                               in_=pd[0:NKF, :, :], func=AF.Copy), reads=[kbk], writes=['U'])
        for nb in range(8):
            bk, kbk = bank(2 + nb % 2)
            py = bk[:, :].rearrange("p (j a) -> p j a", j=16)
            for j in range(16):
                n2 = nb * 16 + j
                k.op('pe', lambda e: e.matmul(py[0:GCH, j, :], lhsT=D_sb[0:NKF, :, 0, n2], rhs=mre[0:NKF, n2, :], start=True, stop=False),
                     reads=['U', 'mre'], writes=[kbk])
                k.op('pe', lambda e: e.matmul(py[0:GCH, j, :], lhsT=D_sb[0:NKF, :, 1, n2], rhs=mim[0:NKF, n2, :], start=False, stop=True),
                     reads=['U', 'mim'], writes=[kbk])
            k.op('act', lambda e: e.activation(out=yout[:, :].rearrange("p (a n) -> p n a", n=128)[:, nb * 16:(nb + 1) * 16, :], in_=py[0:GCH, :, :], func=AF.Copy),
                 reads=[kbk], writes=['yout'])
            if g + 1 < NG:
                build_batch(g + 1, 2 * nb)
                build_batch(g + 1, 2 * nb + 1)
        if g + 1 < NG:
            build_stats(g + 1)
        k.dma('act', 'yco', T['yc'][c0:c0 + GCH, :], yout[:], reads=['yout'], writes=['yc_d'])
    k.end()


PHASES.update(phase3=phase3)


def phase4(k, T):
    k.begin()
    identf = k.sbuf('identf', [128, 128], F32)
    ident = k.sbuf('ident', [128, 128], BF16)
    s1p = k.sbuf('s1p', [128, 8], F32)
    sh1p = k.sbuf('sh1p', [128, 8], F32)
    cs1p = k.sbuf('cs1p', [128, 8], F32)
    csh1p = k.sbuf('csh1p', [128, 8], F32)
    qr = k.sbuf('qr', [128, 4, LO], BF16)
    kr = k.sbuf('kr', [128, NEXT], BF16)
    vt = k.sbuf('vt', [128, 34, 2, 65], BF16)
    kctx = k.sbuf('kctx', [128, 256], BF16)
    vctx = k.sbuf('vctx', [128, 2, 2, 65], BF16)
    xt = [k.sbuf('xt%d' % i, [128, D], F32) for i in range(2)]
    xn = [k.sbuf('xn%d' % i, [128, D], BF16) for i in range(2)]
    st = [k.sbuf('st%d' % i, [128, 2, 6], F32) for i in range(2)]
    mv = [k.sbuf('mv%d' % i, [128, 2], F32) for i in range(2)]
    rstd = [k.sbuf('rstd%d' % i, [128, 1], F32) for i in range(2)]
    banks = [k.psum('bk%d' % i, [128, 512], F32) for i in range(6)]
    pTb = [k.psum('pT%d' % i, [128, 8, 128], BF16) for i in range(2)]

    def bank(i):
        return banks[i], ('bk', i)

    k.dma('sp', 'identf', identf[:], T['ident'], writes=['identf'])
    k.op('act', lambda e: e.activation(out=ident[:], in_=identf[:], func=AF.Copy), reads=['identf'], writes=['ident'])
    load_pvec(k, sh1p[:], T['modv'][0, 0:D], 'sh1p')
    load_pvec(k, s1p[:], T['modv'][0, D:2 * D], 's1p')
    load_pvec(k, csh1p[:], T['modv'][1, 0:D], 'csh1p')
    load_pvec(k, cs1p[:], T['modv'][1, D:2 * D], 'cs1p')
    k.op('dve', lambda e: e.tensor_scalar(out=s1p[:], in0=s1p[:], scalar1=1.0, scalar2=None, op0=ALU.add), reads=['s1p'], writes=['s1p'])
    k.op('dve', lambda e: e.tensor_scalar(out=cs1p[:], in0=cs1p[:], scalar1=1.0, scalar2=None, op0=ALU.add), reads=['cs1p'], writes=['cs1p'])
    k.op('pool', lambda e: e.memset(vt[:].rearrange('p a b c -> p (a b c)'), 1.0), writes=['vt'])
    k.op('pool', lambda e: e.memset(vctx[:].rearrange('p a b c -> p (a b c)'), 1.0), writes=['vctx'])

    tcount = [0]

    def hT_a(src_rows):
        sl = tcount[0] % 2
        tcount[0] += 1
        k.dma('sp', ('xt', sl), xt[sl][:], src_rows, writes=[('xt', sl)])
        ln_tile(k, xt[sl], xn[sl], st[sl], mv[sl], rstd[sl], ('xt', sl), sl)
        return sl

    def hT_b(sl):
        for kc in range(8):
            k.op('pe', lambda e: e.transpose(out=pTb[sl][:, kc, :], in_=xn[sl][:, kc * 128:(kc + 1) * 128], identity=ident[:]),
                 reads=[('xn', sl), 'ident'], writes=[('pT', sl)])

    def hT_c(sl, dst, scp, shp, kdst):
        for kc in range(8):
            k.op('act', lambda e: e.activation(out=dst[:, kc, :], in_=pTb[sl][:, kc, :], func=AF.Identity,
                                               scale=scp[:, kc:kc + 1], bias=shp[:, kc:kc + 1]),
                 reads=[('pT', sl), 's1p', 'sh1p', 'cs1p', 'csh1p'], writes=[kdst])

    def make_hT(src_rows, dst, scp, shp, kdst):
        sl = hT_a(src_rows)
        hT_b(sl)
        hT_c(sl, dst, scp, shp, kdst)

    outer = k.pes
    k.pes = ExitStack()
    k.pes.__enter__()
    NW = 1920
    wB = k.sbuf('wB', [128, 8, NW], BF16)
    wG = k.sbuf('wG', [128, 8, 2048], BF16)
    wst = [k.sbuf('wst%d' % i, [128, 8, 128], F32) for i in range(3)]
    cw = k.sbuf('cw', [128, 12, 4], F32)
    hT = [k.sbuf('hT%d' % i, [128, 8, 512], BF16) for i in range(2)]
    ub = [k.sbuf('ub%d' % i, [128, 4, 514], F32) for i in range(2)]
    tc = k.sbuf('tc', [128, 4, 512], F32)
    ycs = [k.sbuf('ycs%d' % i, [128, 4, 512], BF16) for i in range(2)]
    gsb = k.sbuf('gsb', [128, 8, 512], BF16)
    rc = k.sbuf('rc', [128, 512], F32)
    rs = k.sbuf('rs', [128, 512], F32)
    t1 = k.sbuf('t1', [128, 512], F32)
    t2 = k.sbuf('t2', [128, 512], F32)
    k.dma('sp', 'cw', cw[:], T['cw'], writes=['cw'])
    hmk = k.sbuf('hmk', [128, 2], F32)
    k.dma('sp', 'hmk', hmk[:], T['hmask'], writes=['hmk'])
    wn_ = [0]

    def stage_cast(dst, col0):
        i = wn_[0] % 3
        wn_[0] += 1
        k.dma('sp', ('wst', i), wst[i][:], T['w_in_p'][:, col0:col0 + 128].rearrange("(kc p) n -> p kc n", p=128), writes=[('wst', i)])
        if i % 2 == 0:
            k.op('act', lambda e: e.activation(out=dst, in_=wst[i][:], func=AF.Copy), reads=[('wst', i)], writes=['wB', 'wG'])
        else:
            k.op('dve', lambda e: e.tensor_copy(out=dst, in_=wst[i][:]), reads=[('wst', i)], writes=['wB', 'wG'])
    for j0 in (1536, 1664, 1792, 0, 128, 256, 384, 512, 640, 768, 896, 1024, 1152, 1280, 1408):
        stage_cast(wB[:, :, j0:j0 + 128], C_X0 + j0)
    for j0 in range(0, 2048, 128):
        stage_cast(wG[:, :, j0:j0 + 128], C_G + j0)
    OX0, OQ, OQS, OK_, OKS, OV = 0, 512, 1024, 1536, 1664, 1792

    hc = hT[0]
    for j in range(2):
        make_hT(T['ctx'][j * 128:(j + 1) * 128, :], hc[:, :, j * 128:(j + 1) * 128], cs1p, csh1p, ('hT', 0))
    bk, kbk = bank(0)
    for kc in range(8):
        k.op('pe', lambda e: e.matmul(bk[:, 0:256], lhsT=wB[:, kc, OK_:OK_ + 128], rhs=hc[:, kc, 0:256], start=(kc == 0), stop=(kc == 7)),
             reads=['wB', ('hT', 0)], writes=[kbk])
    k.op('act', lambda e: e.activation(out=kctx[:], in_=bk[:, 0:256], func=AF.Copy), reads=[kbk], writes=['kctx'])
    for j in range(2):
        bk, kbk = bank(1 + j)
        for kc in range(8):
            k.op('pe', lambda e: e.matmul(bk[:, 0:128], lhsT=hc[:, kc, j * 128:(j + 1) * 128], rhs=wB[:, kc, OV:OV + 128], start=(kc == 0), stop=(kc == 7)),
                 reads=['wB', ('hT', 0)], writes=[kbk])
        k.op('act', lambda e: e.activation(out=vctx[:, j, :, 0:64], in_=bk[:, 0:128].rearrange("p (h d) -> p h d", h=2), func=AF.Copy),
             reads=[kbk], writes=['vctx'])

    k.op('pool', lambda e: e.memset(ub[1][:, :, 0:2], 0.0), writes=[('ub', 1, c_) for c_ in range(4)])
    ycv = T['yc'].rearrange("(cc p) t -> p cc t", p=128)
    gtv = T['gt'].rearrange("(cc p) t -> p cc t", p=128)
    bi = [0]

    def nbank():
        bi[0] = (bi[0] + 1) % 6
        return bank(bi[0])

    for ci in range(9):
        s = (ci + 1) % 2
        W = 512 if ci < 8 else 256
        e0 = 4 * ci
        if ci == 0:
            for j in range(W // 128):
                make_hT(T['xext'][(e0 + j) * 128:(e0 + j + 1) * 128, :], hT[s][:, :, j * 128:(j + 1) * 128], s1p, sh1p, ('hT', s))
        k.dma('sp', 'rc', rc[:, 0:W], T['ropec'][:, ci * 512:ci * 512 + W], writes=['rc'])
        k.dma('sp', 'rs', rs[:, 0:W], T['ropes'][:, ci * 512:ci * 512 + W], writes=['rs'])

        def proj(col0, bk, kbk):
            for kc in range(8):
                k.op('pe', lambda e: e.matmul(bk[:, 0:W], lhsT=wB[:, kc, col0:col0 + 128], rhs=hT[s][:, kc, 0:W], start=(kc == 0), stop=(kc == 7)),
                     reads=['wB', ('hT', s)], writes=[kbk])

        o_lo = max(0, 512 * ci - 129)
        o_hi = min(LO, 512 * ci + W - 129)
        j_lo = o_lo - (512 * ci - 129)
        j_hi = o_hi - (512 * ci - 129)
        k.dma('sp', ('ycs', s), ycs[s][:, :, 0:o_hi - o_lo], ycv[:, :, o_lo:o_hi], reads=['yc_d'], writes=[('ycs', s)])
        for cc in range(4):
            bk, kbk = nbank()
            proj(OX0 + cc * 128, bk, kbk)
            k.op('act', lambda e: e.activation(out=ub[s][:, cc, 2:2 + W], in_=bk[:, 0:W], func=AF.Copy), reads=[kbk], writes=[('ub', s, cc)])
            if ci == 0:
                k.op('dve', lambda e: e.tensor_scalar(out=ub[s][:, cc, 2:130], in0=ub[s][:, cc, 2:130], scalar1=hmk[:, 0:1], scalar2=None, op0=ALU.mult),
                     reads=[('ub', s, cc), 'hmk'], writes=[('ub', s, cc)])
            if ci == 8:
                k.op('dve', lambda e: e.tensor_scalar(out=ub[s][:, cc, 130:258], in0=ub[s][:, cc, 130:258], scalar1=hmk[:, 1:2], scalar2=None, op0=ALU.mult),
                     reads=[('ub', s, cc), 'hmk'], writes=[('ub', s, cc)])
            k.op('act', lambda e: e.activation(out=tc[:, cc, 0:W], in_=ub[s][:, cc, 1:1 + W], func=AF.Identity,
                                               scale=cw[:, 8 + cc, 1:2], bias=cw[:, 8 + cc, 3:4]), reads=[('ub', s, cc), 'cw'], writes=[('tc', cc)])
            k.op('dve', lambda e: e.scalar_tensor_tensor(out=tc[:, cc, 0:W], in0=ub[s][:, cc, 0:W], scalar=cw[:, 8 + cc, 0:1], in1=tc[:, cc, 0:W],
                                                         op0=ALU.mult, op1=ALU.add), reads=[('ub', s, cc), 'cw', ('tc', cc)], writes=[('tc', cc)])
            k.op('dve', lambda e: e.scalar_tensor_tensor(out=tc[:, cc, 0:W], in0=ub[s][:, cc, 2:2 + W], scalar=cw[:, 8 + cc, 2:3], in1=tc[:, cc, 0:W],
                                                         op0=ALU.mult, op1=ALU.add), reads=[('ub', s, cc), 'cw', ('tc', cc)], writes=[('tc', cc)])
        k.op('pool', lambda e: e.tensor_tensor(out=ycs[s][:, :, 0:o_hi - o_lo], in0=ycs[s][:, :, 0:o_hi - o_lo], in1=tc[:, :, j_lo:j_hi], op=ALU.mult),
             reads=[('ycs', s)] + [('tc', c_) for c_ in range(4)], writes=[('ycs', s)])
        k.dma('sp', ('yho', s), ycv[:, :, o_lo:o_hi], ycs[s][:, :, 0:o_hi - o_lo], reads=[('ycs', s)], writes=['yc_d'])
        if ci < 8:
            k.op('pool', lambda e: e.tensor_copy(out=ub[1 - s][:, :, 0:2], in_=ub[s][:, :, 512:514]), reads=[('ub', s, c_) for c_ in range(4)], writes=[('ub', 1 - s, c_) for c_ in range(4)])

        def rope(col_a, col_b, dst, kdst, w_lo, w_hi):
            ba, kba = nbank()
            proj(col_a, ba, kba)
            bb_, kbb = nbank()
            proj(col_b, bb_, kbb)
            k.op('dve', lambda e: e.tensor_tensor(out=t1[:, 0:W], in0=ba[:, 0:W], in1=rc[:, 0:W], op=ALU.mult), reads=[kba, 'rc'], writes=['t1'])
            k.op('dve', lambda e: e.tensor_tensor(out=t2[:, 0:W], in0=bb_[:, 0:W], in1=rs[:, 0:W], op=ALU.mult), reads=[kbb, 'rs'], writes=['t2'])
            k.op('pool', lambda e: e.tensor_tensor(out=dst, in0=t1[:, w_lo:w_hi], in1=t2[:, w_lo:w_hi], op=ALU.add), reads=['t1', 't2'], writes=[kdst])

        q_lo = max(0, 512 * ci - 128)
        q_hi = min(LO, 512 * ci + W - 128)
        w_lo = q_lo - (512 * ci - 128)
        w_hi = q_hi - (512 * ci - 128)
        for cc in range(4):
            rope(OQ + cc * 128, OQS + cc * 128, qr[:, cc, q_lo:q_hi], 'qr', w_lo, w_hi)
        rope(OK_, OKS, kr[:, ci * 512:ci * 512 + W], 'kr', 0, W)
        for j in range(W // 128):
            bk, kbk = nbank()
            for kc in range(8):
                k.op('pe', lambda e: e.matmul(bk[:, 0:128], lhsT=hT[s][:, kc, j * 128:(j + 1) * 128], rhs=wB[:, kc, OV:OV + 128], start=(kc == 0), stop=(kc == 7)),
                     reads=['wB', ('hT', s)], writes=[kbk])
            k.op('act', lambda e: e.activation(out=vt[:, e0 + j, :, 0:64], in_=bk[:, 0:128].rearrange("p (h d) -> p h d", h=2), func=AF.Copy),
                 reads=[kbk], writes=['vt'])
        Wn = 0 if ci == 8 else (512 if ci + 1 < 8 else 256)
        for hf in range(2):
            for cc in range(8):
                u = hf * 8 + cc
                pj = u // 4 if (u % 4 == 0 and (u // 4) * 128 < Wn) else None
                if pj is not None:
                    slp = hT_a(T['xext'][(e0 + 4 + pj) * 128:(e0 + 5 + pj) * 128, :])
                bk, kbk = nbank()
                for kc in range(8):
                    k.op('pe', lambda e: e.matmul(bk[:, 0:W], lhsT=wG[:, kc, (hf * 8 + cc) * 128:(hf * 8 + cc + 1) * 128], rhs=hT[s][:, kc, 0:W],
                                                  start=(kc == 0), stop=(kc == 7)), reads=['wG', ('hT', s)], writes=[kbk])
                if pj is not None:
                    hT_b(slp)
                k.op('act', lambda e: e.activation(out=gsb[:, cc, 0:W], in_=bk[:, 0:W], func=AF.Sigmoid), reads=[kbk], writes=['gsb'])
                if pj is not None:
                    hT_c(slp, hT[1 - s][:, :, pj * 128:(pj + 1) * 128], s1p, sh1p, ('hT', 1 - s))
            k.dma('act', 'gto', gtv[:, hf * 8:(hf + 1) * 8, q_lo:q_hi], gsb[:, :, w_lo:w_hi], reads=['gsb'], writes=['gt_d'])
    if DEBUG:
        k.dma('sp', 'dq1', T['dbg_qr'], qr[:].rearrange("p a b -> p (a b)"), reads=['qr'], writes=['dq1'])
        k.dma('sp', 'dq2', T['dbg_kr'], kr[:], reads=['kr'], writes=['dq2'])
        k.dma('sp', 'dq3', T['dbg_vt'], vt[:].rearrange("p a b c -> p (a b c)"), reads=['vt'], writes=['dq3'])
        k.dma('sp', 'dq4', T['dbg_kc'], kctx[:], reads=['kctx'], writes=['dq4'])
        k.dma('sp', 'dq5', T['dbg_vc'], vctx[:].rearrange("p a b c -> p (a b c)"), reads=['vctx'], writes=['dq5'])
    k.barrier()
    k.pes.__exit__(None, None, None)
    k.pes = outer

    wbh = k.sbuf('wbh', [128, 4, D], BF16)
    wba = k.sbuf('wba', [128, 4, D], BF16)
    wo = k.sbuf('wo', [128, 8, D], BF16)
    wst2 = [k.sbuf('wst2_%d' % i, [128, 4, 256], F32) for i in range(2)]
    w2n = [0]
    msk = k.sbuf('msk', [128, 4, 512], BF16)
    mskf = k.sbuf('mskf', [128, 4, 512], F32)
    esk = k.sbuf('esk', [128, 8], F32)
    g1b = k.sbuf('g1b', [128, D], F32)
    lng = k.sbuf('lng', [128, D], F32)
    lnb = k.sbuf('lnb', [128, D], F32)
    E = [k.sbuf('E%d' % i, [128, 512], BF16) for i in range(6)]
    osb = k.sbuf('osb', [128, 8, 64], BF16)
    den = k.sbuf('den', [128, 4], F32)
    yat = k.sbuf('yat', [128, 4, 512], BF16)
    yh = [k.sbuf('yh%d' % i, [128, 4, 512], BF16) for i in range(2)]
    gts = [k.sbuf('gts%d' % i, [128, 16, 512], BF16) for i in range(2)]
    mT = k.sbuf('mT', [128, 8, 512], BF16)
    m1 = k.sbuf('m1', [128, 512], F32)
    m2 = k.sbuf('m2', [128, 512], F32)
    zt = k.sbuf('zt', [128, D], F32)
    zn = k.sbuf('zn', [128, D], F32)

    def load_w(dst, src, nk):
        for kc2 in range(0, nk, 4):
            for hf in range(4):
                i2 = w2n[0] % 2
                w2n[0] += 1
                k.dma('sp', ('wst2', i2), wst2[i2][:], src[kc2 * 128:(kc2 + 4) * 128, hf * 256:(hf + 1) * 256].rearrange("(kc p) n -> p kc n", p=128), writes=[('wst2', i2)])
                if i2 == 0:
                    k.op('act', lambda e: e.activation(out=dst[:, kc2:kc2 + 4, hf * 256:(hf + 1) * 256], in_=wst2[i2][:], func=AF.Copy), reads=[('wst2', i2)], writes=['w2'])
                else:
                    k.op('dve', lambda e: e.tensor_copy(out=dst[:, kc2:kc2 + 4, hf * 256:(hf + 1) * 256], in_=wst2[i2][:]), reads=[('wst2', i2)], writes=['w2'])
    deferred = [lambda: load_w(wbh, T['w_bh'], 4), lambda: load_w(wba, T['w_ba'], 4), lambda: load_w(wo, T['w_o'], 8)]
    k.dma('sp', 'mskf', mskf[:], T['masks'], writes=['mskf'])
    k.op('act', lambda e: e.activation(out=msk[:], in_=mskf[:], func=AF.Copy), reads=['mskf'], writes=['msk'])
    k.dma('sp', 'esk', esk[:], T['sinks'], writes=['esk'])
    k.op('act', lambda e: e.activation(out=esk[:], in_=esk[:], func=AF.Exp), reads=['esk'], writes=['esk'])
    k.dma('sp', 'g1b', g1b[:], T['modv'][0:1, 2 * D:3 * D].broadcast_to([128, D]), writes=['g1b'])
    k.dma('sp', 'lng', lng[:], T['ln1'][0:1, :].broadcast_to([128, D]), writes=['lng'])
    k.dma('sp', 'lnb', lnb[:], T['ln1'][1:2, :].broadcast_to([128, D]), writes=['lnb'])

    ei = [0]
    osbs = [osb, k.sbuf('osb1', [128, 8, 64], BF16)]

    def attn_tile(oc, tj):
        t = oc * 4 + tj
        e = t + 1
        ob, kob = osbs[t % 2], ('osb', t % 2)
        for G in range(2):
            ps = slice(64 * G, 64 * G + 64)
            q4 = qr[ps, :, t * 128:(t + 1) * 128]
            blocks = [('w', e - 1, 2 if t == 0 else 0), ('w', e, None), ('w', e + 1, 3 if t == 31 else 1), ('c', 0, None), ('c', 1, None)]
            Es = []
            for (kind, idx, mi) in blocks:
                bk, kbk = bank(ei[0] % 3)
                Eb, kE = E[ei[0] % 6], ('E', ei[0] % 6)
                ei[0] += 1
                kk_ = kr[ps, idx * 128:(idx + 1) * 128] if kind == 'w' else kctx[ps, idx * 128:(idx + 1) * 128]
                k.op('pe', lambda e_: e_.matmul(bk[:, :], lhsT=kk_, rhs=q4, start=True, stop=(mi is None)),
                     reads=['kr', 'kctx', 'qr'], writes=[kbk])
                if mi is not None:
                    k.op('pe', lambda e_: e_.matmul(bk[:, :], lhsT=ident[:], rhs=msk[:, mi, :], start=False, stop=True),
                         reads=['ident', 'msk'], writes=[kbk])
                k.op('act', lambda e_: e_.activation(out=Eb[:], in_=bk[:, :], func=AF.Exp, scale=0.125), reads=[kbk], writes=[kE])
                Es.append((Eb, kE, kind, idx))
            bo, kbo = bank(3 + G)
            po = bo[:, 0:4 * 80].rearrange("p (j x) -> p j x", j=4)
            for j in range(4):
                for bi_, (Eb, kE, kind, idx) in enumerate(Es):
                    vv = vt[:, idx, G, :] if kind == 'w' else vctx[:, idx, G, :]
                    k.op('pe', lambda e_: e_.matmul(po[:, j, 0:65], lhsT=Eb[:, j * 128:(j + 1) * 128], rhs=vv, start=(bi_ == 0), stop=(bi_ == 4)),
                         reads=[kE, 'vt', 'vctx'], writes=[kbo])
            k.op('dve', lambda e_: e_.tensor_tensor(out=den[:, 4 * G:4 * G + 4], in0=po[:, :, 64], in1=esk[:, 4 * G:4 * G + 4], op=ALU.add),
                 reads=[kbo, 'esk'], writes=[('den', G)])
            k.op('dve', lambda e_: e_.reciprocal(out=den[:, 4 * G:4 * G + 4], in_=den[:, 4 * G:4 * G + 4]), reads=[('den', G)], writes=[('den', G)])
            for j in range(4):
                k.op('act', lambda e_: e_.activation(out=ob[:, 4 * G + j, :], in_=po[:, j, 0:64], func=AF.Identity, scale=den[:, 4 * G + j:4 * G + j + 1]),
                     reads=[kbo, ('den', G)], writes=[kob])

        def fin():
            for kc in range(4):
                k.op('pe', lambda e_: e_.transpose(out=pTb[0][:, kc, :], in_=ob[:, 2 * kc:2 * kc + 2, :].rearrange("p h d -> p (h d)"), identity=ident[:]),
                     reads=[kob, 'ident'], writes=[('pT', 0)])
            k.op('dve', lambda e_: e_.tensor_copy(out=yat[:, :, tj * 128:(tj + 1) * 128], in_=pTb[0][:, 0:4, :]), reads=[('pT', 0)], writes=['yat'])
        return fin

    def branch(oc):
        s = oc % 2
        for dc in range(8):
            bh, kbh = bank(5)
            ba, kba = bank(4 - (dc % 2))
            for kc in range(4):
                k.op('pe', lambda e_: e_.matmul(bh[:, :], lhsT=wbh[:, kc, dc * 128:(dc + 1) * 128], rhs=yh[s][:, kc, :], start=(kc == 0), stop=(kc == 3)),
                     reads=['w2', ('yh', s)], writes=[kbh])
            for kc in range(4):
                k.op('pe', lambda e_: e_.matmul(ba[:, :], lhsT=wba[:, kc, dc * 128:(dc + 1) * 128], rhs=yat[:, kc, :], start=(kc == 0), stop=(kc == 3)),
                     reads=['w2', 'yat'], writes=[kba])
            k.op('dve', lambda e_: e_.tensor_tensor(out=m1[:], in0=bh[:, :], in1=gts[s][:, dc, :], op=ALU.mult), reads=[kbh, ('gts', s)], writes=['m1'])
            k.op('dve', lambda e_: e_.tensor_tensor(out=m2[:], in0=ba[:, :], in1=gts[s][:, 8 + dc, :], op=ALU.mult), reads=[kba, ('gts', s)], writes=['m2'])
            k.op('dve', lambda e_: e_.tensor_tensor(out=mT[oc % 2][:, dc, :], in0=m1[:], in1=m2[:], op=ALU.add), reads=['m1', 'm2'], writes=['mT'])

    def outproj_tile(oc, tj):
        t = oc * 4 + tj
        sl = t % 2
        k.dma('sp', ('xt', sl), xt[sl][:], T['xext'][(t + 1) * 128:(t + 2) * 128, :], writes=[('xt', sl)])
        for hf in range(2):
            bk, kbk = bank(5 if hf == 0 else 4)
            for kc in range(8):
                k.op('pe', lambda e_: e_.matmul(bk[:, :], lhsT=mT[oc % 2][:, kc, tj * 128:(tj + 1) * 128], rhs=wo[:, kc, hf * 512:(hf + 1) * 512],
                                                start=(kc == 0), stop=(kc == 7)), reads=['mT', 'w2'], writes=[kbk])
            k.op('dve', lambda e_: e_.tensor_tensor(out=zt[:, hf * 512:(hf + 1) * 512], in0=bk[:, :], in1=g1b[:, hf * 512:(hf + 1) * 512], op=ALU.mult),
                 reads=[kbk, 'g1b'], writes=['zt'])
        k.op('dve', lambda e_: e_.scalar_tensor_tensor(out=zt[:], in0=xt[sl][:], scalar=ALPHA, in1=zt[:], op0=ALU.mult, op1=ALU.add),
             reads=[('xt', sl), 'zt'], writes=['zt'])
        ln_tile(k, zt, zns[sl], st[sl], mv[sl], rstd[sl], 'zt', sl, kout=('zn', sl))
        k.op('dve', lambda e_: e_.tensor_tensor(out=zns[sl][:], in0=zns[sl][:], in1=lng[:], op=ALU.mult), reads=[('zn', sl), 'lng'], writes=[('zn', sl)])
        k.op('dve', lambda e_: e_.tensor_tensor(out=zns[sl][:], in0=zns[sl][:], in1=lnb[:], op=ALU.add), reads=[('zn', sl), 'lnb'], writes=[('zn', sl)])
        k.dma('sp', ('x1o', sl), T['x1'][t * 128:(t + 1) * 128, :], zns[sl][:], reads=[('zn', sl)], writes=['x1_d'])

    mT = [mT, mT]
    zns = [zn, k.sbuf('zn1', [128, D], F32)]
    den = k.sbuf('den8', [128, 8], F32)
    pending = []
    for oc in range(8):
        s = oc % 2
        k.dma('sp', ('yh', s), yh[s][:], ycv[:, :, oc * 512:(oc + 1) * 512], reads=['yc_d'], writes=[('yh', s)])
        k.dma('sp', ('gts', s), gts[s][:], gtv[:, :, oc * 512:(oc + 1) * 512], reads=['gt_d'], writes=[('gts', s)])
        prev_fin = None
        for tj in range(4):
            fin = attn_tile(oc, tj)
            if deferred:
                deferred.pop(0)()
            if prev_fin is not None:
                prev_fin()
            if pending:
                outproj_tile(*pending.pop(0))
            prev_fin = fin
        prev_fin()
        branch(oc)
        pending = [(oc, tj) for tj in range(4)]
    while pending:
        outproj_tile(*pending.pop(0))
    k.end()


PHASES.update(phase4=phase4)


def phase5(k, T):
    k.begin()
    identf = k.sbuf('identf', [128, 128], F32)
    ident = k.sbuf('ident', [128, 128], BF16)
    s2p = k.sbuf('s2p', [128, 8], F32)
    sh2p = k.sbuf('sh2p', [128, 8], F32)
    g2b = k.sbuf('g2b', [128, D], F32)
    lng = k.sbuf('lng', [128, D], F32)
    lnb = k.sbuf('lnb', [128, D], F32)
    bgr = k.sbuf('bgr', [128, 20], F32)
    self_ = k.sbuf('self', [16, 16, 128], F32)
    sel = k.sbuf('sel', [16, 16, 128], BF16)
    wgrf = k.sbuf('wgrf', [128, 8, 20], F32)
    wgr = k.sbuf('wgr', [128, 8, 20], BF16)
    wd = k.sbuf('wd', [128, NE, 2, D], BF16)
    hid = k.sbuf('hid', [128, NE, 2, 512], BF16)
    t2T = [k.sbuf('t2T%d' % i, [128, 8, 512], BF16) for i in range(2)]
    gT = [k.sbuf('gT%d' % i, [16, 512], BF16) for i in range(2)]
    wgb = [k.sbuf('wgb%d' % i, [128, 8, DE], BF16) for i in range(3)]
    wub = [k.sbuf('wub%d' % i, [128, 8, DE], BF16) for i in range(3)]
    xt = [k.sbuf('xt%d' % i, [128, D], F32) for i in range(2)]
    xn = [k.sbuf('xn%d' % i, [128, D], BF16) for i in range(2)]
    st = [k.sbuf('st%d' % i, [128, 2, 6], F32) for i in range(2)]
    mv = [k.sbuf('mv%d' % i, [128, 2], F32) for i in range(2)]
    rstd = [k.sbuf('rstd%d' % i, [128, 1], F32) for i in range(2)]
    xr = [k.sbuf('xr%d' % i, [128, D], F32) for i in range(2)]
    zt = k.sbuf('zt', [128, D], F32)
    zn = [k.sbuf('zn%d' % i, [128, D], F32) for i in range(2)]
    sg = [k.sbuf('sg%d' % i, [128, 512], F32) for i in range(2)]
    tm = [k.sbuf('tm%d' % i, [128, 512], F32) for i in range(2)]
    lg = k.sbuf('lg', [128, 20], F32)
    sm = k.sbuf('sm', [128, 16], F32)
    oh = k.sbuf('oh', [128, 4], F32)
    ig = k.sbuf('ig', [128, 4], F32)
    ig2 = k.sbuf('ig2', [128, 4], F32)
    oh1 = k.sbuf('oh1', [128, 4], F32)
    oh2 = k.sbuf('oh2', [128, 4], F32)
    eg = k.sbuf('eg', [128, 4], F32)
    ws = k.sbuf('ws', [128, 4], F32)
    g16 = k.sbuf('g16', [128, 16], F32)
    banks = [k.psum('bk%d' % i, [128, 512], F32) for i in range(7)]
    pTb = [k.psum('pT%d' % i, [128, 8, 128], BF16) for i in range(1)]

    def bank(i):
        return banks[i], ('bk', i)

    k.dma('sp', 'identf', identf[:], T['ident'], writes=['identf'])
    k.op('act', lambda e: e.activation(out=ident[:], in_=identf[:], func=AF.Copy), reads=['identf'], writes=['ident'])
    load_pvec(k, sh2p[:], T['modv'][0, 3 * D:4 * D], 'sh2p')
    load_pvec(k, s2p[:], T['modv'][0, 4 * D:5 * D], 's2p')
    k.op('dve', lambda e: e.tensor_scalar(out=s2p[:], in0=s2p[:], scalar1=1.0, scalar2=None, op0=ALU.add), reads=['s2p'], writes=['s2p'])
    k.dma('sp', 'g2b', g2b[:], T['modv'][0:1, 5 * D:6 * D].broadcast_to([128, D]), writes=['g2b'])
    k.dma('sp', 'lng', lng[:], T['ln2'][0:1, :].broadcast_to([128, D]), writes=['lng'])
    k.dma('sp', 'lnb', lnb[:], T['ln2'][1:2, :].broadcast_to([128, D]), writes=['lnb'])
    k.dma('sp', 'bgr', bgr[:], T['b_gr'], writes=['bgr'])
    k.dma('sp', 'self', self_[:], T['sel'], writes=['self'])
    k.op('act', lambda e: e.activation(out=sel[:], in_=self_[:], func=AF.Copy), reads=['self'], writes=['sel'])
    k.dma('sp', 'wgrf', wgrf[:], T['w_gr'].rearrange("(kc p) n -> p kc n", p=128), writes=['wgrf'], slow=True)
    k.op('act', lambda e: e.activation(out=wgr[:], in_=wgrf[:], func=AF.Copy), reads=['wgrf'], writes=['wgr'])

    tcount = [0]
    R = ['lg', 'sm', 'oh', 'ig', 'ig2', 'oh1', 'oh2', 'eg', 'ws']

    pst = {}

    def prep_a(sc, j):
        sl = tcount[0] % 2
        tcount[0] += 1
        t = sc * 4 + j
        k.dma('sp', ('xt', sl), xt[sl][:], T['x1'][t * 128:(t + 1) * 128, :], reads=['x1_d'], writes=[('xt', sl)])
        ln_tile(k, xt[sl], xn[sl], st[sl], mv[sl], rstd[sl], ('xt', sl), sl)
        pst[(sc, j)] = sl

    def prep_b(sc, j):
        bf = sc % 2
        sl = pst[(sc, j)]
        for kc in range(8):
            k.op('pe', lambda e: e.transpose(out=pTb[0][:, kc, :], in_=xn[sl][:, kc * 128:(kc + 1) * 128], identity=ident[:]),
                 reads=[('xn', sl), 'ident'], writes=['pT'])
        for kc in range(8):
            k.op('act', lambda e: e.activation(out=t2T[bf][:, kc, j * 128:(j + 1) * 128], in_=pTb[0][:, kc, :], func=AF.Identity,
                                               scale=s2p[:, kc:kc + 1], bias=sh2p[:, kc:kc + 1]),
                 reads=['pT', 's2p', 'sh2p'], writes=[('t2T', bf)])

    def prep_c(sc, j):
        bf = sc % 2
        bk, kbk = bank(6)
        for kc in range(8):
            k.op('pe', lambda e: e.matmul(bk[:, 0:20], lhsT=t2T[bf][:, kc, j * 128:(j + 1) * 128], rhs=wgr[:, kc, :], start=(kc == 0), stop=(kc == 7)),
                 reads=[('t2T', bf), 'wgr'], writes=[kbk])

        def dv(fn, reads=R, writes=R):
            k.op('dve', fn, reads=reads, writes=writes)
        dv(lambda e: e.tensor_tensor(out=lg[:], in0=bk[:, 0:20], in1=bgr[:], op=ALU.add), reads=[kbk, 'bgr'] + R)
        el = lg[:, 4:20].rearrange("p (g e) -> p g e", g=4)
        dv(lambda e: e.tensor_reduce(out=sm[:, 0:1], in_=lg[:, 0:4], axis=AX.X, op=ALU.max))
        dv(lambda e: e.tensor_scalar(out=oh[:], in0=lg[:, 0:4], scalar1=sm[:, 0:1], scalar2=None, op0=ALU.is_equal))
        dv(lambda e: e.tensor_scalar(out=eg[:], in0=lg[:, 0:4], scalar1=sm[:, 0:1], scalar2=None, op0=ALU.subtract))
        k.op('act', lambda e: e.activation(out=eg[:], in_=eg[:], func=AF.Exp), reads=R, writes=R)
        dv(lambda e: e.tensor_reduce(out=sm[:, 1:2], in_=eg[:], axis=AX.X, op=ALU.add))
        dv(lambda e: e.reciprocal(out=sm[:, 1:2], in_=sm[:, 1:2]))
        dv(lambda e: e.tensor_scalar(out=ig[:], in0=el[:, 0, :], scalar1=oh[:, 0:1], scalar2=None, op0=ALU.mult))
        for g in range(1, 4):
            dv(lambda e: e.scalar_tensor_tensor(out=ig[:], in0=el[:, g, :], scalar=oh[:, g:g + 1], in1=ig[:], op0=ALU.mult, op1=ALU.add))
        dv(lambda e: e.tensor_reduce(out=sm[:, 2:3], in_=ig[:], axis=AX.X, op=ALU.max))
        dv(lambda e: e.tensor_scalar(out=oh1[:], in0=ig[:], scalar1=sm[:, 2:3], scalar2=None, op0=ALU.is_equal))
        dv(lambda e: e.scalar_tensor_tensor(out=ig2[:], in0=oh1[:], scalar=-1e30, in1=ig[:], op0=ALU.mult, op1=ALU.add))
        dv(lambda e: e.tensor_reduce(out=sm[:, 3:4], in_=ig2[:], axis=AX.X, op=ALU.max))
        dv(lambda e: e.tensor_scalar(out=oh2[:], in0=ig2[:], scalar1=sm[:, 3:4], scalar2=None, op0=ALU.is_equal))
        dv(lambda e: e.tensor_tensor(out=sm[:, 4:5], in0=sm[:, 3:4], in1=sm[:, 2:3], op=ALU.subtract))
        k.op('act', lambda e: e.activation(out=sm[:, 5:6], in_=sm[:, 4:5], func=AF.Exp), reads=R, writes=R)
        dv(lambda e: e.tensor_scalar(out=sm[:, 6:7], in0=sm[:, 5:6], scalar1=1.0, scalar2=None, op0=ALU.add))
        dv(lambda e: e.reciprocal(out=sm[:, 6:7], in_=sm[:, 6:7]))
        dv(lambda e: e.tensor_tensor(out=sm[:, 7:8], in0=sm[:, 6:7], in1=sm[:, 1:2], op=ALU.mult))
        dv(lambda e: e.tensor_tensor(out=sm[:, 8:9], in0=sm[:, 7:8], in1=sm[:, 5:6], op=ALU.mult))
        dv(lambda e: e.tensor_scalar(out=ws[:], in0=oh1[:], scalar1=sm[:, 7:8], scalar2=None, op0=ALU.mult))
        dv(lambda e: e.scalar_tensor_tensor(out=ws[:], in0=oh2[:], scalar=sm[:, 8:9], in1=ws[:], op0=ALU.mult, op1=ALU.add))
        for g in range(4):
            k.op('dve', lambda e: e.tensor_scalar(out=g16[:, 4 * g:4 * g + 4], in0=ws[:], scalar1=oh[:, g:g + 1], scalar2=None, op0=ALU.mult),
                 reads=R, writes=['g16'])

    def prep_d(sc, j):
        bf = sc % 2
        bk, kbk = bank(6)
        k.op('pe', lambda e: e.transpose(out=bk[0:16, 128:256], in_=g16[:], identity=identf[:]), reads=['g16', 'identf'], writes=[kbk])
        k.op('act', lambda e: e.activation(out=gT[bf][:, j * 128:(j + 1) * 128], in_=bk[0:16, 128:256], func=AF.Copy), reads=[kbk], writes=[('gT', bf)])

    wn = [0]

    def load_expert(e):
        i = wn[0] % 3
        wn[0] += 1
        k.dma('sp', ('wgb', i), wgb[i][:].rearrange("p a b -> p (a b)"), T['wgu'][e, 0], writes=[('wgb', i)])
        k.dma('sp', ('wub', i), wub[i][:].rearrange("p a b -> p (a b)"), T['wgu'][e, 1], writes=[('wub', i)])
        return i

    for j in range(4):
        prep_a(0, j)
        prep_b(0, j)
        prep_c(0, j)
        prep_d(0, j)
    for e in range(NE):
        k.dma('sp', 'wd', wd[:, e, :, :].rearrange("p a b -> p (a b)"), T['wdb'][e], writes=['wd'])
    pend = [load_expert(0)]
    fcn = [0]
    for sc in range(8):
        bf = sc % 2
        for e in range(NE):
            wi = pend.pop(0)
            nxt = (sc * NE + e + 1)
            if nxt < 8 * NE:
                pend.append(load_expert(nxt % NE))
            bw, kbw = bank(4 + e % 2)
            k.op('pe', lambda en: en.matmul(bw[:, :], lhsT=sel[:, e, :], rhs=gT[bf][:, :], start=True, stop=True), reads=['sel', ('gT', bf)], writes=[kbw])
            pj = (e - 1) // 4 if (sc < 7 and e % 4 == 1) else None
            dj = (e - 2) // 4 if (sc < 7 and e % 4 == 2) else None
            if pj is not None:
                prep_a(sc + 1, pj)
            for fc in range(2):
                q = fcn[0] % 2
                fcn[0] += 1
                bg, kbg = bank(q)
                bu, kbu = bank(2 + q)
                for kc in range(8):
                    k.op('pe', lambda en: en.matmul(bg[:, :], lhsT=wgb[wi][:, kc, fc * 128:(fc + 1) * 128], rhs=t2T[bf][:, kc, :], start=(kc == 0), stop=(kc == 7)),
                         reads=[('wgb', wi), ('t2T', bf)], writes=[kbg])
                for kc in range(8):
                    k.op('pe', lambda en: en.matmul(bu[:, :], lhsT=wub[wi][:, kc, fc * 128:(fc + 1) * 128], rhs=t2T[bf][:, kc, :], start=(kc == 0), stop=(kc == 7)),
                         reads=[('wub', wi), ('t2T', bf)], writes=[kbu])
                k.op('act', lambda en: en.activation(out=sg[q][:], in_=bg[:, :], func=AF.Silu), reads=[kbg], writes=[('sg', q)])
                k.op('dve', lambda en: en.tensor_tensor(out=tm[q][:], in0=sg[q][:], in1=bu[:, :], op=ALU.mult), reads=[('sg', q), kbu], writes=[('tm', q)])
                k.op('dve', lambda en: en.tensor_tensor(out=hid[:, e, fc, :], in0=tm[q][:], in1=bw[:, :], op=ALU.mult), reads=[('tm', q), kbw], writes=['hid'])
                if pj is not None and fc == 0:
                    prep_b(sc + 1, pj)
                if pj is not None and fc == 1:
                    prep_c(sc + 1, pj)
            if dj is not None:
                prep_d(sc + 1, dj)
        for j in range(4):
            t = sc * 4 + j
            sl = t % 2
            k.dma('sp', ('xr', sl), xr[sl][:], T['x1'][t * 128:(t + 1) * 128, :], reads=['x1_d'], writes=[('xr', sl)])
            for hf in range(2):
                q = fcn[0] % 2
                fcn[0] += 1
                bk, kbk = bank(2 * (hf % 2) + q) if False else bank(q + 2 * hf)
                n = 0
                for e in range(NE):
                    for fc in range(2):
                        k.op('pe', lambda en: en.matmul(bk[:, :], lhsT=hid[:, e, fc, j * 128:(j + 1) * 128], rhs=wd[:, e, fc, hf * 512:(hf + 1) * 512],
                                                        start=(n == 0), stop=(n == 2 * NE - 1)), reads=['hid', 'wd'], writes=[kbk])
                        n += 1
                k.op('dve', lambda en: en.tensor_tensor(out=zt[:, hf * 512:(hf + 1) * 512], in0=bk[:, :], in1=g2b[:, hf * 512:(hf + 1) * 512], op=ALU.mult),
                     reads=[kbk, 'g2b'], writes=['zt'])
            k.op('dve', lambda en: en.scalar_tensor_tensor(out=zt[:], in0=xr[sl][:], scalar=ALPHA, in1=zt[:], op0=ALU.mult, op1=ALU.add),
                 reads=[('xr', sl), 'zt'], writes=['zt'])
            ln_tile(k, zt, zn[sl], st[sl], mv[sl], rstd[sl], 'zt', sl, kout=('zn', sl))
            k.op('pool', lambda en: en.tensor_tensor(out=zn[sl][:], in0=zn[sl][:], in1=lng[:], op=ALU.mult), reads=[('zn', sl), 'lng'], writes=[('zn', sl)])
            k.op('pool', lambda en: en.tensor_tensor(out=zn[sl][:], in0=zn[sl][:], in1=lnb[:], op=ALU.add), reads=[('zn', sl), 'lnb'], writes=[('zn', sl)])
            k.dma('sp', ('outo', sl), T['out'][t * 128:(t + 1) * 128, :], zn[sl][:], reads=[('zn', sl)], writes=['out_d'])
    k.end()


PHASES.update(phase5=phase5)


_NC_CACHE = {}


def kernel(**inputs):
    maps = _host_inputs(inputs)
    if 'nc' not in _NC_CACHE:
        _NC_CACHE['nc'] = build_nc()
    nc = _NC_CACHE['nc']
    res = run_bass_kernel_spmd(nc, maps, core_ids=list(range(8)))
    out = np.empty((4, L, D), np.float32)
    for core in range(8):
        b, half = core // 2, core % 2
        out[b, half * LO:(half + 1) * LO] = np.asarray(res.results[core]['out'])
    return out
```
